# Optimizing a Trainium2 kernel written in Bass

```python
import jax
import jax.numpy as jnp
from jax import lax
import numpy as np


D_MODEL = 1024
BATCH = 16
SEQ = 2048
DEPTH = 1

GRID_W = 64
CTX_LEN = 256
NA_HEADS = 8
NA_HEAD_DIM = 64
NA_WIDTH = NA_HEADS * NA_HEAD_DIM
NA_KH = 8
NA_KW = 16
NA_QBW = 16
NA_KBW = NA_QBW + NA_KW
ROPE_AXIS_DIM = NA_HEAD_DIM // 2
ROPE_BASE = 10000.0
HG_HEADS = 4
HG_DK = 128
HG_DV = 128
HG_WIDTH = HG_HEADS * HG_DK
HG_CHUNK = 64
MIX_WIDTH = NA_WIDTH + HG_WIDTH
SEG_WIDTHS = (NA_WIDTH, NA_WIDTH, NA_WIDTH, HG_WIDTH, HG_WIDTH, HG_WIDTH, HG_WIDTH, HG_WIDTH)
IN_COLS = 3 * NA_WIDTH + 5 * HG_WIDTH
N_GROUPS = 4
EXPERTS_PER_GROUP = 4
N_EXPERTS = N_GROUPS * EXPERTS_PER_GROUP
TOP_K = 2
D_EXPERT = 512
EPS = 1e-6
F32 = jnp.float32

kernel_name = 'hybrid_na_hgrn2_hmoe_dit_layer'


def _rms(x, g):
    xf = x.astype(F32)
    y = xf * lax.rsqrt(jnp.mean(xf * xf, axis=-1, keepdims=True) + EPS)
    return (y * g.astype(F32)).astype(x.dtype)


def _seg(w, i):
    start = sum(SEG_WIDTHS[:i])
    return w[:, start:start + SEG_WIDTHS[i]]


def _axial_rope(n_tok):
    t = jnp.arange(n_tok)
    pos = jnp.stack([t // GRID_W, t % GRID_W], axis=-1).astype(F32)
    inv = ROPE_BASE ** (-jnp.arange(0, ROPE_AXIS_DIM, 2, dtype=F32) / ROPE_AXIS_DIM)
    ang = pos[:, :, None] * inv
    return jnp.cos(ang), jnp.sin(ang)


def _rope(x, cos, sin):
    xr = x.astype(F32).reshape(*x.shape[:-1], 2, ROPE_AXIS_DIM)
    half = ROPE_AXIS_DIM // 2
    x1, x2 = xr[..., :half], xr[..., half:]
    cs, sn = cos[None, :, None], sin[None, :, None]
    out = jnp.concatenate([x1 * cs - x2 * sn, x2 * cs + x1 * sn], axis=-1)
    return out.reshape(x.shape).astype(x.dtype)


def _na_column_tables():
    n_cb = GRID_W // NA_QBW
    j = np.arange(n_cb)
    kb_start = np.clip(j * NA_QBW - NA_KW // 2, 0, GRID_W - NA_KBW)
    key_cols = kb_start[:, None] + np.arange(NA_KBW)
    q_cols = j[:, None] * NA_QBW + np.arange(NA_QBW)
    cs = np.clip(q_cols - NA_KW // 2, 0, GRID_W - NA_KW)[..., None]
    kc = key_cols[:, None, :]
    valid = (kc >= cs) & (kc < cs + NA_KW)
    col_off = np.clip(kc - q_cols[..., None] + NA_KW - 1, 0, 2 * NA_KW - 2)
    return key_cols, valid, col_off


def _na_latent(q_rot, k_rot, v, q_raw, kc, vc, rpb):
    B, S, H, Dh = v.shape
    rows = S // GRID_W
    kh = min(NA_KH, rows)
    key_cols, valid, col_off = _na_column_tables()
    n_cb = key_cols.shape[0]
    grid = lambda t: t.reshape(B, rows, GRID_W, H, Dh)
    qg, kg, vg, qrg = grid(q_rot), grid(k_rot), grid(v), grid(q_raw)
    scale = Dh ** -0.5
    mask = jnp.asarray(valid)[None, None, :, :, None, :]

    def one_row(r):
        rs = jnp.clip(r - kh // 2, 0, rows - kh)
        k_blk = lax.dynamic_slice_in_dim(kg, rs, kh, axis=1)[:, :, key_cols]
        v_blk = lax.dynamic_slice_in_dim(vg, rs, kh, axis=1)[:, :, key_cols]
        qb = lax.dynamic_index_in_dim(qg, r, axis=1, keepdims=False).reshape(B, n_cb, NA_QBW, H, Dh)
        qrb = lax.dynamic_index_in_dim(qrg, r, axis=1, keepdims=False).reshape(B, n_cb, NA_QBW, H, Dh)
        s_loc = jnp.einsum('bjqhd,bijkhd->bhjqik', qb, k_blk).astype(F32) * scale
        row_off = rs + jnp.arange(kh) - r + NA_KH - 1
        bias = rpb[:, row_off][:, :, col_off].transpose(0, 2, 3, 1, 4)
        s_loc = jnp.where(mask, s_loc + bias[None].astype(F32), -jnp.inf)
        s_loc = s_loc.reshape(B, H, n_cb, NA_QBW, kh * NA_KBW)
        s_ctx = jnp.einsum('bjqhd,bchd->bhjqc', qrb, kc).astype(F32) * scale
        p = jax.nn.softmax(jnp.concatenate([s_loc, s_ctx], axis=-1), axis=-1).astype(v.dtype)
        p_loc = p[..., :kh * NA_KBW].reshape(B, H, n_cb, NA_QBW, kh, NA_KBW)
        p_ctx = p[..., kh * NA_KBW:]
        o = (jnp.einsum('bhjqik,bijkhd->bjqhd', p_loc, v_blk)
             + jnp.einsum('bhjqc,bchd->bjqhd', p_ctx, vc))
        return o.reshape(B, GRID_W, H, Dh)

    out = lax.map(one_row, jnp.arange(rows))
    return out.transpose(1, 0, 2, 3, 4).reshape(B, S, H * Dh)


def _ctx_attn(q, k, v):
    B, L, H, Dh = q.shape
    s = jnp.einsum('bqhd,bkhd->bhqk', q, k).astype(F32) * (Dh ** -0.5)
    p = jax.nn.softmax(s, axis=-1).astype(v.dtype)
    return jnp.einsum('bhqk,bkhd->bqhd', p, v).reshape(B, L, H * Dh)


def _hg_heads(t):
    B, L, _ = t.shape
    return t.astype(F32).reshape(B, L, HG_HEADS, -1).transpose(0, 2, 1, 3)


def _hg_forget(f_pre, lb):
    f = lb + (1.0 - lb) * jax.nn.sigmoid(f_pre.astype(F32))
    return _hg_heads(1.0 - f), _hg_heads(jnp.log(f))


def _flip(t):
    return jnp.flip(t, axis=2)


def _hg_scan(q, k, v, logf, s0):
    B, H, L, DK = q.shape
    n = L // HG_CHUNK
    chunks = lambda t: jnp.moveaxis(t.reshape(B, H, n, HG_CHUNK, t.shape[-1]), 2, 0)
    incl = jnp.tril(jnp.ones((HG_CHUNK, HG_CHUNK), dtype=bool))[:, :, None]

    def step(S, inp):
        qc, kc, vc, gc = inp
        b = jnp.cumsum(gc, axis=2)
        diff = b[:, :, :, None, :] - b[:, :, None, :, :]
        decay = jnp.exp(jnp.where(incl, diff, -jnp.inf))
        attn = jnp.einsum('bhtd,bhsd,bhtsd->bhts', qc, kc, decay)
        o = (jnp.einsum('bhtd,bhdv->bhtv', qc * jnp.exp(b), S)
             + jnp.einsum('bhts,bhsv->bhtv', attn, vc))
        b_end = b[:, :, -1:, :]
        S = (jnp.exp(b_end[:, :, 0, :])[..., None] * S
             + jnp.einsum('bhsd,bhsv->bhdv', kc * jnp.exp(b_end - b), vc))
        return S, o

    S, o = lax.scan(step, s0, (chunks(q), chunks(k), chunks(v), chunks(logf)))
    return jnp.moveaxis(o, 0, 2).reshape(B, H, L, v.shape[-1]), S


def _hg_final_state(k, v, logf):
    b = jnp.cumsum(logf, axis=2)
    return jnp.einsum('bhsd,bhsv->bhdv', k * jnp.exp(b[:, :, -1:, :] - b), v)


def _hg_output(o, g_pre, gain):
    o = o.transpose(0, 2, 1, 3)
    y = o * lax.rsqrt(jnp.mean(o * o, axis=-1, keepdims=True) + EPS) * gain.astype(F32)
    g = g_pre.astype(F32).reshape(*g_pre.shape[:-1], HG_HEADS, HG_DV)
    B, L = g_pre.shape[:2]
    return (y * jax.nn.silu(g)).reshape(B, L, HG_WIDTH).astype(g_pre.dtype)


def _hier_moe(h, w_grp, b_grp, w_exp, b_exp, w1, w3, w2):
    B, L, D = h.shape
    t = h.reshape(B * L, D)
    g_logits = (t @ w_grp + b_grp).astype(F32)
    g_prob = jax.nn.softmax(g_logits, axis=-1)
    g_sel = jnp.argmax(g_logits, axis=-1)
    g_w = jnp.take_along_axis(g_prob, g_sel[:, None], axis=-1)
    e_logits = (t @ w_exp + b_exp).astype(F32).reshape(-1, N_GROUPS, EXPERTS_PER_GROUP)
    e_in = jnp.take_along_axis(e_logits, g_sel[:, None, None], axis=1)[:, 0]
    top_v, top_i = lax.top_k(e_in, TOP_K)
    w_top = jax.nn.softmax(top_v, axis=-1) * g_w
    e_id = g_sel[:, None] * EXPERTS_PER_GROUP + top_i
    gate = jnp.sum(jax.nn.one_hot(e_id, N_EXPERTS, dtype=F32) * w_top[..., None], axis=1).astype(h.dtype)
    out = jnp.zeros_like(t)
    for e in range(N_EXPERTS):
        y = (jax.nn.silu(t @ w1[e]) * (t @ w3[e])) @ w2[e]
        out = out + gate[:, e:e + 1] * y
    return out.reshape(B, L, D)


def setup_inputs(seed: int = 0) -> dict:
    key = jax.random.key(seed)
    ks = jax.random.split(key, 22)
    nrm = lambda k, shape, s: jax.random.normal(k, shape, F32) * s
    D = D_MODEL
    return {
        'x': nrm(ks[0], (BATCH, SEQ, D), 1.0),
        'c': nrm(ks[1], (BATCH, D), 1.0),
        'ctx': nrm(ks[2], (BATCH, CTX_LEN, D), 1.0),
        'c_ctx': nrm(ks[3], (D,), 1.0),
        'w_mod': nrm(ks[4], (DEPTH, D, 6 * D), 0.5 * D ** -0.5),
        'b_mod': nrm(ks[5], (DEPTH, 6 * D), 0.01),
        'norm_mix': 1.0 + nrm(ks[6], (DEPTH, D), 0.05),
        'norm_ffn': 1.0 + nrm(ks[7], (DEPTH, D), 0.05),
        'w_in': nrm(ks[8], (DEPTH, D, IN_COLS), D ** -0.5),
        'w_out': nrm(ks[9], (DEPTH, MIX_WIDTH, D), MIX_WIDTH ** -0.5),
        'na_rpb': nrm(ks[10], (DEPTH, NA_HEADS, 2 * NA_KH - 1, 2 * NA_KW - 1), 0.5),
        'hg_lb': nrm(ks[11], (DEPTH + 1, 2, HG_WIDTH), 0.5),
        'hg_norm': 1.0 + nrm(ks[12], (DEPTH, HG_DV), 0.05),
        'w_grp': nrm(ks[13], (DEPTH, D, N_GROUPS), D ** -0.5),
        'b_grp': nrm(ks[14], (DEPTH, N_GROUPS), 0.01),
        'w_exp': nrm(ks[15], (DEPTH, D, N_EXPERTS), D ** -0.5),
        'b_exp': nrm(ks[16], (DEPTH, N_EXPERTS), 0.01),
        'w1': nrm(ks[17], (DEPTH, N_EXPERTS, D, D_EXPERT), D ** -0.5),
        'w3': nrm(ks[18], (DEPTH, N_EXPERTS, D, D_EXPERT), D ** -0.5),
        'w2': nrm(ks[19], (DEPTH, N_EXPERTS, D_EXPERT, D), D_EXPERT ** -0.5),
        'norm_final': 1.0 + nrm(ks[20], (D,), 0.05),
    }


def reference(x, c, ctx, c_ctx, w_mod, b_mod, norm_mix, norm_ffn, w_in, w_out, na_rpb,
              hg_lb, hg_norm, w_grp, b_grp, w_exp, b_exp, w1, w3, w2, norm_final):
    B, S, _ = x.shape
    cos, sin = _axial_rope(S)
    lb_all = jnp.cumsum(jax.nn.softmax(hg_lb.astype(F32), axis=0), axis=0)
    splits = [int(s) for s in np.cumsum(SEG_WIDTHS)[:-1]]
    na_heads = lambda t: t.reshape(*t.shape[:-1], NA_HEADS, NA_HEAD_DIM)
    xc = ctx
    for l in range(DEPTH):
        last = l == DEPTH - 1
        mod = jax.nn.silu(c) @ w_mod[l] + b_mod[l]
        sh_a, sc_a, ga_a, sh_f, sc_f, ga_f = jnp.split(mod[:, None, :], 6, axis=-1)
        mod_c = jax.nn.silu(c_ctx) @ w_mod[l] + b_mod[l]
        csh_a, csc_a, cga_a, csh_f, csc_f, cga_f = jnp.split(mod_c, 6)

        h = _rms(x, norm_mix[l]) * (1.0 + sc_a) + sh_a
        hc = _rms(xc, norm_mix[l]) * (1.0 + csc_a) + csh_a
        na_q, na_k, na_v, hg_q, hg_i, hg_ff, hg_fb, hg_g = jnp.split(h @ w_in[l], splits, axis=-1)
        if last:
            c_k, c_v, c_i, c_ff, c_fb = [hc @ _seg(w_in[l], i) for i in (1, 2, 4, 5, 6)]
        else:
            c_q, c_k, c_v, c_hq, c_i, c_ff, c_fb, c_g = jnp.split(hc @ w_in[l], splits, axis=-1)

        q_a = na_heads(na_q)
        na_out = _na_latent(_rope(q_a, cos, sin), _rope(na_heads(na_k), cos, sin), na_heads(na_v),
                            q_a, na_heads(c_k), na_heads(c_v), na_rpb[l])

        lb_f, lb_b = lb_all[l, 0], lb_all[l, 1]
        lq, li = _hg_heads(jax.nn.silu(hg_q)), _hg_heads(hg_i)
        lkf, lgf = _hg_forget(hg_ff, lb_f)
        lkb, lgb = _hg_forget(hg_fb, lb_b)
        ci = _hg_heads(c_i)
        ckf, cgf = _hg_forget(c_ff, lb_f)
        ckb, cgb = _hg_forget(c_fb, lb_b)
        if last:
            s_f = _hg_final_state(ckf, ci, cgf)
            s_b = _hg_final_state(_flip(ckb), _flip(ci), _flip(cgb))
        else:
            cq = _hg_heads(jax.nn.silu(c_hq))
            zero = jnp.zeros((B, HG_HEADS, HG_DK, HG_DV), F32)
            co_f, s_f = _hg_scan(cq, ckf, ci, cgf, zero)
            co_b, s_b = _hg_scan(_flip(cq), _flip(ckb), _flip(ci), _flip(cgb), zero)
        o_f, _ = _hg_scan(lq, lkf, li, lgf, s_f)
        o_b, _ = _hg_scan(_flip(lq), _flip(lkb), _flip(li), _flip(lgb), s_b)
        hg_out = _hg_output(o_f + _flip(o_b), hg_g, hg_norm[l])

        x = x + ga_a * (jnp.concatenate([na_out, hg_out], axis=-1) @ w_out[l])
        if not last:
            ctx_na = _ctx_attn(na_heads(c_q), na_heads(c_k), na_heads(c_v))
            ctx_hg = _hg_output(co_f + _flip(co_b), c_g, hg_norm[l])
            xc = xc + cga_a * (jnp.concatenate([ctx_na, ctx_hg], axis=-1) @ w_out[l])

        h = _rms(x, norm_ffn[l]) * (1.0 + sc_f) + sh_f
        x = x + ga_f * _hier_moe(h, w_grp[l], b_grp[l], w_exp[l], b_exp[l], w1[l], w3[l], w2[l])
        if not last:
            hc = _rms(xc, norm_ffn[l]) * (1.0 + csc_f) + csh_f
            xc = xc + cga_f * _hier_moe(hc, w_grp[l], b_grp[l], w_exp[l], b_exp[l], w1[l], w3[l], w2[l])

    return _rms(x, norm_final)
```

```python
import numpy as np
from contextlib import ExitStack
import concourse.bass as bass
import concourse.mybir as mybir
from concourse.bass_utils import run_bass_kernel_spmd

F32 = mybir.dt.float32
BF16 = mybir.dt.bfloat16
AF = mybir.ActivationFunctionType
ALU = mybir.AluOpType
AX = mybir.AxisListType

D = 1024
SEQ = 2048
LC = 256
NT = SEQ // 128
EPS = 1e-6
NPAT = 21
SB_BASE = 16512
SB_END = 229376


class Src:
    def __init__(self, name, sem, step):
        self.name, self.sem, self.step, self.cnt = name, sem, step, 0


class Buf:
    def __init__(self, name=""):
        self.name = name
        self.w = {}
        self.r = {}
        self.dsem = None
        self.excl = False


class Sched:
    def __init__(self, nc, es):
        self.nc, self.es = nc, es
        self.eng, self.q = {}, {}
        for n in ("pe", "act", "dve", "pool", "sp"):
            self.eng[n] = Src(n, es.enter_context(nc.semaphore("s_" + n)), 1)
            self.q[n] = []
        self.seen = {n: {} for n in self.eng}
        self.dsrcs = []
        self.same_engine_sync = True

    def _waits(self, en, reads, writes):
        deps = {}
        E_ = self.eng[en]
        for b in reads:
            for s, v in b.w.items():
                if deps.get(s, 0) < v:
                    deps[s] = v
            if b.excl:
                for s, v in b.r.items():
                    if s is not E_ and deps.get(s, 0) < v:
                        deps[s] = v
        for b in writes:
            for d in (b.w, b.r):
                for s, v in d.items():
                    if deps.get(s, 0) < v:
                        deps[s] = v
        E = self.eng[en]
        out = []
        for s, v in deps.items():
            if s is E and (en in ("pe", "sp") or not self.same_engine_sync):
                continue
            if self.seen[en].get(s, 0) >= v:
                continue
            self.seen[en][s] = v
            out.append((s.sem, v))
        return out

    def op(self, en, fn, reads=(), writes=()):
        E = self.eng[en]
        waits = self._waits(en, reads, writes)
        E.cnt += 1
        self.q[en].append((waits, fn, (E.sem, 1)))
        for b in reads:
            b.r[E] = E.cnt
        for b in writes:
            b.w = {E: E.cnt}
            b.r = {}

    def dma(self, en, dst_buf, src_buf, fn):
        track = dst_buf if dst_buf is not None else src_buf
        if track.dsem is None:
            sem = self.es.enter_context(self.nc.semaphore("d%d" % len(self.dsrcs)))
            track.dsem = Src("dma_" + track.name, sem, 16)
            self.dsrcs.append(track.dsem)
        Dm = track.dsem
        reads = [src_buf] if src_buf is not None else []
        writes = [dst_buf] if dst_buf is not None else []
        waits = self._waits(en, reads, writes)
        Dm.cnt += 16
        self.q[en].append((waits, fn, (Dm.sem, 16)))
        for b in reads:
            b.r[Dm] = Dm.cnt
        for b in writes:
            b.w = {Dm: Dm.cnt}
            b.r = {}
        return Dm

    def barrier(self):
        srcs = list(self.eng.values()) + self.dsrcs
        for en in self.eng:
            waits = []
            for s in srcs:
                if (s is self.eng[en] and en in ("pe", "sp")) or s.cnt == 0:
                    continue
                if self.seen[en].get(s, 0) >= s.cnt:
                    continue
                self.seen[en][s] = s.cnt
                waits.append((s.sem, s.cnt))
            if waits:
                self.q[en].append((waits, None, None))

    def emit(self):
        nc = self.nc
        with nc.Block() as block:
            def mk(en):
                def body(e):
                    for waits, fn, inc in self.q[en]:
                        for sem, v in waits:
                            e.wait_ge(sem, v)
                        if fn is not None:
                            fn(e).then_inc(inc[0], inc[1])
                return body
            block.tensor(mk("pe"))
            block.scalar(mk("act"))
            block.vector(mk("dve"))
            block.gpsimd(mk("pool"))
            block.sync(mk("sp"))


class Mem:
    def __init__(self, nc):
        self.nc = nc
        self.free = [(SB_BASE, SB_END)]
        self.pending = []
        self.n = 0

    def alloc(self, shape, dt, name="t"):
        per = 1
        for s in shape[1:]:
            per *= s
        nbytes = per * (2 if dt == BF16 else 4)
        nbytes = (nbytes + 63) // 64 * 64
        for i, (a, b) in enumerate(self.free):
            if b - a >= nbytes:
                self.free[i] = (a + nbytes, b)
                self.n += 1
                key = (a, tuple(shape), str(dt))
                self._cache = getattr(self, "_cache", {})
                if key in self._cache:
                    t = self._cache[key]
                else:
                    t = self.nc.alloc_sbuf_tensor_at("%s_%d" % (name, self.n), list(shape), dt, offset=a)
                    self._cache[key] = t
                t_rng = (a, a + nbytes)
                self._rng = getattr(self, "_rng", {})
                self._rng[id(t)] = t_rng
                self._keep = getattr(self, "_keep", [])
                self._keep.append(t)
                return t
        raise RuntimeError("SBUF OOM for %s %s; free=%s" % (name, shape, self.free))

    def release(self, *ts):
        for t in ts:
            self.pending.append(self._rng[id(t)])

    def commit(self):
        fr = self.free + self.pending
        self.pending = []
        fr = sorted([f for f in fr if f[1] > f[0]])
        out = []
        for a, b in fr:
            if out and out[-1][1] == a:
                out[-1] = (out[-1][0], b)
            else:
                out.append((a, b))
        self.free = out


def _na_patterns():
    pats = [(2, [0, 1, 2, 3, 4]), (0, [0, 1, 2, 3]), (1, [0, 1, 2, 3]), (14, [12, 13, 14, 15]), (15, [12, 13, 14, 15])]
    ridx = np.zeros((NPAT, 128, 128), np.int64)
    cidx = np.zeros((NPAT, 128, 128), np.int64)
    mask = np.zeros((NPAT, 128, 128), np.float32)
    kk = np.arange(128)[:, None]
    qq = np.arange(128)[None, :]
    pos = 0
    for m, chunks in pats:
        for c in chunks:
            kr = 2 * c + kk // 64
            kc = kk % 64
            r = 2 * m + qq // 64
            cq = qq % 64
            rs = np.clip(r - 4, 0, 24)
            cs = np.clip(cq - 8, 0, 48)
            valid = (kr >= rs) & (kr < rs + 8) & (kc >= cs) & (kc < cs + 16)
            ridx[pos] = np.clip(kr - r + 7, 0, 14)
            cidx[pos] = np.clip(kc - cq + 15, 0, 30)
            mask[pos] = valid
            pos += 1
    assert pos == NPAT
    return ridx, cidx, mask


def _pat_of_block(m):
    if 2 <= m <= 13:
        return 0, [m - 2, m - 1, m, m + 1, m + 2]
    if m == 0:
        return 5, [0, 1, 2, 3]
    if m == 1:
        return 9, [0, 1, 2, 3]
    if m == 14:
        return 13, [12, 13, 14, 15]
    return 17, [12, 13, 14, 15]


def _rope_tables():
    t = np.arange(SEQ)
    pos = np.stack([t // 64, t % 64], axis=-1).astype(np.float32)
    inv = (10000.0 ** (-np.arange(0, 32, 2, dtype=np.float32) / 32)).astype(np.float32)
    ang = pos[:, :, None] * inv
    cos, sin = np.cos(ang).astype(np.float32), np.sin(ang).astype(np.float32)
    C = np.zeros((64, SEQ), np.float32)
    Sg = np.zeros((64, SEQ), np.float32)
    for d in range(64):
        a, j = d // 32, d % 32
        if j < 16:
            C[d] = cos[:, a, j]
            Sg[d] = -sin[:, a, j]
        else:
            C[d] = cos[:, a, j - 16]
            Sg[d] = sin[:, a, j - 16]
    return np.concatenate([C, C], 0), np.concatenate([Sg, Sg], 0)


def _perm64():
    p = np.zeros(64, np.int64)
    for d in range(64):
        a, j = d // 32, d % 32
        p[d] = a * 32 + (j + 16) % 32
    return p


LIM = None
MARKS = []
SUB = 9


def build(NB=2, dbg=None, stop=None):
    dbg = dbg or []
    nc = bass.Bass("TRN2", target_bir_lowering=False)
    es = ExitStack()
    with es:
        S = Sched(nc, es)
        M = Mem(nc)
        dram = lambda n, s, kind="ExternalInput": nc.dram_tensor(n, list(s), F32, kind=kind).ap()
        x_d = dram("x", [NB, SEQ, D])
        ctx_d = dram("ctx", [NB, LC, D])
        cT_d = dram("cT", [128, 8, 3])
        wmod_d = dram("w_mod", [D, 6 * D])
        bmodT_d = dram("b_modT", [128, 48])
        bmodrow_d = dram("b_mod_row", [1, 6 * D])
        nmixT_d = dram("nmixT", [128, 8])
        nffnT_d = dram("nffnT", [128, 8])
        nfin_d = dram("nfin_row", [1, D])
        wing_d = dram("w_ing", [8, D, 640])
        wout_d = dram("w_out", [D, D])
        biasg_d = dram("biasg", [4, 128, 2 * NPAT * 128])
        maskE_d = dram("maskE", [128, NPAT * 128])
        ropeC_d = dram("ropeC", [128, SEQ])
        ropeS_d = dram("ropeS", [128, SEQ])
        lbT_d = dram("lbT", [128, 16])
        hgn_d = dram("hgnT", [128, 1])
        wr_d = dram("wr", [D, 20])
        br_d = dram("br_row", [1, 20])
        w1_d = dram("w1", [16, D, 512])
        w3_d = dram("w3", [16, D, 512])
        w2_d = dram("w2", [16, 512, D])
        ident_d = dram("ident", [128, 128])
        tri_d = dram("tri", [64, 128])
        out_d = dram("out", [NB, SEQ, D], kind="ExternalOutput")
        dbg_d = {n: dram("dbg_" + n, s, kind="ExternalOutput") for n, s in dbg}

        PS = [es.enter_context(nc.psum_tensor("ps%d" % i, [128, 1024], F32)) for i in range(4)]
        PB = [Buf("psb%d" % i) for i in range(8)]
        for b_ in PB:
            b_.excl = True
        rr = {"b": 0, "p": 0}

        def bank():
            i = rr["b"]
            rr["b"] = (i + 1) % 8
            return PS[i // 2][:, (i % 2) * 512:(i % 2) * 512 + 512], PB[i]

        def pair():
            i = rr["p"]
            rr["p"] = (i + 1) % 4
            return PS[i], [PB[2 * i], PB[2 * i + 1]]

        def MM(out, lhsT, rhs, start, stop, R, W):
            S.op("pe", lambda e: e.matmul(out, lhsT, rhs, start=start, stop=stop), R, W)

        def ACT(out, in_, func, R, W, bias=None, scale=None, accum=None):
            kw = {}
            if bias is not None:
                kw["bias"] = bias
            if scale is not None:
                kw["scale"] = scale
            if accum is not None:
                kw["accum_out"] = accum
            S.op("act", lambda e: e.activation(out, in_, func, **kw), R, W)

        def TT(en, out, in0, in1, op, R, W):
            S.op(en, lambda e: e.tensor_tensor(out, in0, in1, op), R, W)

        def TS(en, out, in0, s1, s2, op0, op1, R, W):
            if s2 is None:
                S.op(en, lambda e: e.tensor_scalar(out, in0, s1, None, op0), R, W)
            else:
                S.op(en, lambda e: e.tensor_scalar(out, in0, s1, s2, op0, op1), R, W)

        def STT(en, out, in0, sc, in1, op0, op1, R, W):
            S.op(en, lambda e: e.scalar_tensor_tensor(out, in0, sc, in1, op0, op1), R, W)

        def CP(en, out, in_, R, W):
            S.op(en, lambda e: e.tensor_copy(out, in_), R, W)

        def MSET(en, ap, val, W):
            S.op(en, lambda e: e.memset(ap, val), [], W)

        def RECIP(out, in_, R, W):
            S.op("dve", lambda e: e.reciprocal(out, in_), R, W)

        def RMAX(out, in_, R, W):
            S.op("dve", lambda e: e.reduce_max(out, in_, AX.X), R, W)

        def DMA(en, out, in_, dstB, srcB):
            return S.dma(en, dstB, srcB, lambda e: e.dma_start(out=out, in_=in_))

        def phase_end(label=""):
            S.barrier()
            M.commit()
            MARKS.append((label, {n: S.eng[n].cnt for n in S.eng}))

        def dump(name, ap, Bs):
            if name in dbg_d:
                if not isinstance(Bs, list):
                    Bs = [Bs]
                S.barrier()
                for B_ in Bs[1:]:
                    B_.r = dict(B_.r)
                Dsrc = DMA("pool", dbg_d[name], ap, None, Bs[0])
                for B_ in Bs[1:]:
                    B_.r[Dsrc] = Dsrc.cnt

        hT = M.alloc([128, 8, SEQ], BF16, "hT")
        hTB = [Buf("hT%d" % i) for i in range(NT)]
        hcT = M.alloc([128, 8, LC], BF16, "hcT")
        hcTB = [Buf("hcT%d" % i) for i in range(2)]
        ident32 = M.alloc([128, 128], F32, "ident32"); Bid32 = Buf()
        ident16 = M.alloc([128, 128], BF16, "ident16"); Bid16 = Buf()
        ones32 = M.alloc([128, 128], F32, "ones32"); Bon32 = Buf()
        onesm = M.alloc([128, 128], F32, "onesm"); Bonm = Buf()
        ones16 = M.alloc([128, 64], BF16, "ones16"); Bon16 = Buf()
        onesm16 = M.alloc([128, 128], BF16, "onesm16"); Bonm16 = Buf()
        ones128 = M.alloc([128, 128], BF16, "ones128"); Bon128 = Buf()
        tri = M.alloc([64, 128], F32, "tri"); Btri = Buf()
        modT = M.alloc([128, 48, 3], F32, "modT"); BmodT = Buf()
        bmodT = M.alloc([128, 48], F32, "bmodT"); BbmodT = Buf()
        nmixT = M.alloc([128, 8], F32, "nmixT"); Bnmix = Buf()
        nffnT = M.alloc([128, 8], F32, "nffnT"); Bnffn = Buf()
        A_a = M.alloc([128, 8, 3], F32, "A_a"); BA_a = Buf()
        A_f = M.alloc([128, 8, 3], F32, "A_f"); BA_f = Buf()
        lbraw = M.alloc([128, 16], F32, "lbraw"); Blbraw = Buf()
        lbv = M.alloc([128, 8], F32, "lbv"); Blbv = Buf()
        oml = M.alloc([128, 8], F32, "oml"); Boml = Buf()
        hgn = M.alloc([128, 1], F32, "hgn"); Bhgn = Buf()
        wr32 = M.alloc([128, 8, 20], F32, "wr32"); Bwr = Buf()
        brrep = M.alloc([128, 20], F32, "brrep"); Bbr = Buf()
        ssq = M.alloc([128, 64], F32, "ssq"); Bssq = Buf()
        rstd = M.alloc([128, 64], F32, "rstd"); Brstd = Buf()
        Bsc = [Buf("ssqc%d" % i) for i in range(64)]
        Brc = [Buf("rstdc%d" % i) for i in range(64)]
        gate = M.alloc([128, NT, 16], F32, "gate"); BgateT = [Buf("gate%d" % i) for i in range(NT)]
        epsb = M.alloc([128, 1], F32, "epsb"); Bepsb = Buf()

        DMA("sp", ident32[:], ident_d, Bid32, None)
        DMA("pool", ident16[:], ident_d, Bid16, None)
        DMA("sp", tri[:], tri_d, Btri, None)
        DMA("sp", bmodT[:], bmodT_d, BbmodT, None)
        DMA("sp", nmixT[:], nmixT_d, Bnmix, None)
        DMA("sp", nffnT[:], nffnT_d, Bnffn, None)
        DMA("sp", lbraw[:], lbT_d, Blbraw, None)
        DMA("sp", hgn[:], hgn_d, Bhgn, None)
        DMA("sp", wr32[:], wr_d.rearrange("(k p) n -> p k n", p=128), Bwr, None)
        DMA("sp", brrep[:], br_d.partition_broadcast(128), Bbr, None)
        MSET("dve", ones32[:], 1.0, [Bon32])
        MSET("dve", onesm[:], 1.0 / 128.0, [Bonm])
        MSET("dve", ones16[:], 1.0, [Bon16])
        MSET("dve", onesm16[:], 1.0 / 128.0, [Bonm16])
        MSET("dve", ones128[:], 1.0, [Bon128])
        MSET("dve", epsb[:], EPS, [Bepsb])
        lbr = lbraw[:].rearrange("p (a l) -> p a l", l=2)
        TT("dve", lbv[:], lbr[:, :, 0], lbr[:, :, 1], ALU.subtract, [Blbraw], [Blbv])
        ACT(lbv[:], lbv[:], AF.Sigmoid, [Blbv], [Blbv])
        TS("dve", oml[:], lbv[:], -1.0, 1.0, ALU.mult, ALU.add, [Blbv], [Boml])

        scT = M.alloc([128, 8, 3], F32, "scT"); BscT = Buf()
        wst = [M.alloc([128, 8, 1024], F32, "wst") for _ in range(2)]
        Bwst = [Buf("wst0"), Buf("wst1")]
        DMA("sp", scT[:], cT_d, BscT, None)
        ACT(scT[:], scT[:], AF.Silu, [BscT], [BscT])
        brow = M.alloc([3, 6 * D], F32, "brow"); Bbrow = Buf()
        modrow = M.alloc([3, 6 * D], F32, "modrow"); Bmodrow = [Buf() for _ in range(12)]
        DMA("sp", brow[:], bmodrow_d.partition_broadcast(3), Bbrow, None)
        for j in range(6):
            sl = j % 2
            DMA("sp" if j % 2 == 0 else "act", wst[sl][:], wmod_d[:, j * 1024:(j + 1) * 1024].rearrange("(k p) n -> p k n", p=128), Bwst[sl], None)
            for half in range(2):
                pb, pB = bank()
                col0 = j * 1024 + half * 512
                for k in range(8):
                    MM(pb[0:3, :], scT[:, k, :], wst[sl][:, k, half * 512:(half + 1) * 512], k == 0, k == 7, [Bwst[sl], BscT], [pB])
                TT("dve", modrow[0:3, col0:col0 + 512], pb[0:3, :], brow[0:3, col0:col0 + 512], ALU.add, [pB, Bbrow, Bmodrow[2 * j + half]], [Bmodrow[2 * j + half]])
        pT, pTB_ = bank()
        for ch in range(48):
            MM(pT[:, ch * 3:(ch + 1) * 3], modrow[0:3, ch * 128:(ch + 1) * 128], ident32[0:3, 0:3], True, True, [Bmodrow[ch // 4], Bid32], [pTB_])
        CP("dve", modT[:].rearrange("p c n -> p (c n)"), pT[:, 0:144], [pTB_, BmodT], [BmodT])
        for n in range(3):
            STT("dve", A_a[:, :, n], modT[:, 8:16, n], 1.0, nmixT[:], ALU.add, ALU.mult, [BmodT, Bnmix, BA_a], [BA_a])
            STT("dve", A_f[:, :, n], modT[:, 32:40, n], 1.0, nffnT[:], ALU.add, ALU.mult, [BmodT, Bnffn, BA_f], [BA_f])
        dump("modT", modT[:], BmodT)
        M.release(scT, *wst, brow, modrow)
        phase_end("p0")

        def rms_stats(src_ap, col, R):
            ACT(junk[:], src_ap, AF.Square, R + [Bssq, Bsc[col]], [Bsc[col]], accum=ssq[:, col:col + 1])
            ACT(rstd[:, col:col + 1], ssq[:, col:col + 1], AF.Sqrt, [Bsc[col], Bepsb, Brc[col]], [Brc[col]], bias=epsb[:], scale=1.0 / D)
            RECIP(rstd[:, col:col + 1], rstd[:, col:col + 1], [Brc[col]], [Brc[col]])

        def TR(out, in_, R, W):
            S.op("pe", lambda e: e.transpose(out, in_, ident32[:]), R + [Bid32], W)

        def norm_transpose(src_ap, srcB, col, Acol, shcol, n, dst, dstB, eng, dst32=None):
            TS("dve", src_ap[:], src_ap[:], rstd[:, col:col + 1], None, ALU.mult, None, [srcB, Brc[col]], [srcB])
            pp, pBs = pair()
            for k in range(8):
                TR(pp[:, k * 128:(k + 1) * 128], src_ap[:, k * 128:(k + 1) * 128], [srcB], pBs)
            for k in range(8):
                o = dst[k] if dst32 is None else dst32[:, k, :]
                if eng == "dve":
                    TS("dve", o, pp[:, k * 128:(k + 1) * 128], Acol(k, n), shcol(k, n), ALU.mult, ALU.add, pBs + [BA_a, BA_f, BmodT, dstB], [dstB])
                else:
                    ACT(o, pp[:, k * 128:(k + 1) * 128], AF.Identity, pBs + [BA_a, BA_f, BmodT, dstB], [dstB], bias=shcol(k, n), scale=Acol(k, n))

        for b in range(NB):
            mixT = M.alloc([128, 8, SEQ], BF16, "mixT")
            mixB = [[Buf("mix%d_%d" % (c, t)) for t in range(4)] for c in range(8)]
            wg = [M.alloc([128, 8, 640], BF16, "wg") for _ in range(2)]
            Bwg = [Buf("wg0"), Buf("wg1")]
            wgi = [0]

            def load_wg(g):
                sl = wgi[0] % 2
                wgi[0] += 1
                DMA("pool", wg[sl][:], wing_d[g].rearrange("(k p) n -> p k n", p=128), Bwg[sl], None)
                return wg[sl], Bwg[sl]

            ropeC = M.alloc([128, SEQ], F32, "ropeC"); BrC = Buf()
            ropeS = M.alloc([128, SEQ], F32, "ropeS"); BrS = Buf()
            maskE = M.alloc([128, NPAT * 128], BF16, "maskE"); BmE = Buf()
            DMA("sp", ropeC[:], ropeC_d, BrC, None)
            DMA("sp", ropeS[:], ropeS_d, BrS, None)
            DMA("pool", maskE[:], maskE_d, BmE, None)
            qrot = M.alloc([128, 2, SEQ], BF16, "qrot"); Bqrot = [Buf() for _ in range(4)]
            krot = M.alloc([128, SEQ], BF16, "krot"); Bkrot = [Buf() for _ in range(4)]
            qraw = M.alloc([128, 2, SEQ], BF16, "qraw"); Bqraw = [Buf() for _ in range(4)]
            MSET("pool", qrot[:], 0.0, Bqrot)
            MSET("pool", qraw[:], 0.0, Bqraw)
            Vt = M.alloc([128, NT, 128], BF16, "Vt"); BVt = [Buf() for _ in range(4)]
            kctx = M.alloc([128, LC], BF16, "kctx"); Bkctx = Buf()
            vctx = M.alloc([128, 2, 128], BF16, "vctx"); Bvctx = Buf()
            Ebh = [M.alloc([128, NPAT * 128], F32, "Ebh") for _ in range(2)]; BEbh = [Buf(), Buf()]

            def prepE(j_, hh_):
                if j_ >= 4:
                    return
                DMA("sp", Ebh[hh_][:], biasg_d[j_][:, hh_ * NPAT * 128:(hh_ + 1) * NPAT * 128], BEbh[hh_], None)
                ACT(Ebh[hh_][:], Ebh[hh_][:], AF.Exp, [BEbh[hh_]], [BEbh[hh_]])
                for p_ in range(3):
                    sl_ = slice(p_ * 7 * 128, (p_ + 1) * 7 * 128)
                    TT("dve", Ebh[hh_][:, sl_], Ebh[hh_][:, sl_], maskE[:, sl_], ALU.mult, [BEbh[hh_], BmE], [BEbh[hh_]])

            t1 = [M.alloc([128, 512], F32, "t1") for _ in range(2)]; Bt1 = [Buf(), Buf()]
            t2 = [M.alloc([128, 512], F32, "t2") for _ in range(2)]; Bt2 = [Buf(), Buf()]
            P1 = [M.alloc([128, 640], F32, "P1") for _ in range(3)]; BP1 = [Buf() for _ in range(3)]
            Pt = [M.alloc([128, 896], BF16, "Pt") for _ in range(3)]; BPt = [Buf() for _ in range(3)]
            rec = [M.alloc([128, 128], F32, "rec") for _ in range(2)]; Brec = [Buf(), Buf()]
            cnt = {"t": 0, "u": 0}
            nxt = load_wg(0)
            prepE(0, 0)
            prepE(0, 1)
            xst = [M.alloc([128, D], F32, "xst") for _ in range(4)]
            Bxst = [Buf("xst%d" % i) for i in range(4)]
            junk = M.alloc([128, D], BF16, "junk"); Bjunk = Buf()
            Dm = [M.alloc([128, 128], F32, "Dm") for _ in range(3)]
            BDm = [Buf("Dm%d" % i) for i in range(3)]
            MSET("dve", ssq[:], 0.0, [Bssq])

            def p1_stats(i):
                sl = i % 4
                if i < NT:
                    src = x_d[b, i * 128:(i + 1) * 128, :]
                else:
                    src = ctx_d[b, (i - NT) * 128:(i - NT + 1) * 128, :]
                DMA("sp", xst[sl][:], src, Bxst[sl], None)
                rms_stats(xst[sl][:], i, [Bxst[sl]])

            def p1_trans(i):
                sl = i % 4
                if i < NT:
                    n = b
                    dst = [hT[:, k, i * 128:(i + 1) * 128] for k in range(8)]
                    dB = hTB[i]
                else:
                    n = 2
                    dst = [hcT[:, k, (i - NT) * 128:(i - NT + 1) * 128] for k in range(8)]
                    dB = hcTB[i - NT]
                norm_transpose(xst[sl], Bxst[sl], i, lambda k, n: A_a[:, k, n:n + 1], lambda k, n: modT[:, k, n:n + 1], n, dst, dB,
                               "dve" if i % 2 == 0 else "act")

            for i in range(NT + 2 + 2):
                if i < NT + 2:
                    p1_stats(i)
                if i >= 2:
                    p1_trans(i - 2)
            if b == 0:
                dump("hT", hT[:, :, 0:256], [hTB[0], hTB[1]])
                dump("ssq", ssq[:], Bsc[0:18])
                dump("rstd", rstd[:], Brc[0:18])
            M.release(*xst, junk, *Dm)
            if stop == "p1":
                phase_end("p1")
                break
            MARKS.append(("p1", {n: S.eng[n].cnt for n in S.eng}))

            for j in range(4 if LIM is None else 1):
                w, Bw = nxt
                if LIM == 1:
                    break
                for tb in range(4):
                    hr = hTB[4 * tb:4 * tb + 4]
                    tsl = slice(tb * 512, (tb + 1) * 512)
                    for which in range(2):
                        c0 = which * 256
                        pa, pAB = bank()
                        pp_, pPB = bank()
                        for k in range(8):
                            MM(pa, w[:, k, c0:c0 + 128], hT[:, k, tsl], k == 0, k == 7, hr + [Bw], [pAB])
                        for k in range(8):
                            MM(pp_, w[:, k, c0 + 128:c0 + 256], hT[:, k, tsl], k == 0, k == 7, hr + [Bw], [pPB])
                        s = cnt["t"] % 2
                        cnt["t"] += 1
                        TT("dve", t1[s][:], pa, ropeC[:, tsl], ALU.mult, [pAB, BrC, Bt1[s]], [Bt1[s]])
                        TT("dve", t2[s][:], pp_, ropeS[:, tsl], ALU.mult, [pPB, BrS, Bt2[s]], [Bt2[s]])
                        if which == 0:
                            if SUB >= 2:
                                for h2_ in range(2):
                                    hp_ = slice(64 * h2_, 64 * h2_ + 64)
                                    ACT(qraw[hp_, h2_, tsl], pa[hp_, :], AF.Copy, [pAB, Bqraw[tb]], [Bqraw[tb]])
                            if SUB >= 3:
                                for h2_ in range(2):
                                    hp_ = slice(64 * h2_, 64 * h2_ + 64)
                                    TT("pool", qrot[hp_, h2_, tsl], t1[s][hp_, :], t2[s][hp_, :], ALU.add, [Bt1[s], Bt2[s], Bqrot[tb]], [Bqrot[tb]])
                        else:
                            if SUB >= 3:
                                TT("pool", krot[:, tsl], t1[s][:], t2[s][:], ALU.add, [Bt1[s], Bt2[s], Bkrot[tb]], [Bkrot[tb]])
                    if SUB < 4:
                        continue
                    pv, pVB = bank()
                    for ti in range(4):
                        i = 4 * tb + ti
                        for k in range(8):
                            MM(pv[:, ti * 128:(ti + 1) * 128], hT[:, k, i * 128:(i + 1) * 128], w[:, k, 512:640], k == 0, k == 7, [hTB[i], Bw], [pVB])
                    ACT(Vt[:, 4 * tb:4 * tb + 4, :], pv.rearrange("p (t c) -> p t c", c=128), AF.Copy, [pVB, BVt[tb]], [BVt[tb]])
                if SUB < 5:
                    break
                pc, pCB = bank()
                for k in range(8):
                    MM(pc[:, 0:LC], w[:, k, 256:384], hcT[:, k, :], k == 0, k == 7, hcTB + [Bw], [pCB])
                for ti in range(2):
                    for k in range(8):
                        MM(pc[:, 256 + ti * 128:256 + (ti + 1) * 128], hcT[:, k, ti * 128:(ti + 1) * 128], w[:, k, 512:640], k == 0, k == 7, hcTB + [Bw], [pCB])
                ACT(kctx[:], pc[:, 0:LC], AF.Copy, [pCB, Bkctx], [Bkctx])
                ACT(vctx[:], pc[:, 256:512].rearrange("p (t c) -> p t c", c=128), AF.Copy, [pCB, Bvctx], [Bvctx])
                nxt = load_wg(j + 1)
                if LIM == 2:
                    break
                units = [(hh_, m_) for hh_ in range(2 if LIM is None else 1) for m_ in range(16 if LIM is None else LIM - 2)]
                SKN = 2
                ust = {}

                def stageA(u_):
                    hh, m = units[u_]
                    hs = slice(64 * hh, 64 * hh + 64)
                    p0, chunks = _pat_of_block(m)
                    nl = len(chunks)
                    qs_ = slice(m * 128, (m + 1) * 128)
                    qB = [Bqrot[m // 4], Bqraw[m // 4]]
                    pi = u_ % 3
                    pS, pSB = PS[pi], [PB[2 * pi], PB[2 * pi + 1]]
                    for ci, c in enumerate(chunks):
                        MM(pS[:, ci * 128:(ci + 1) * 128], krot[:, c * 128:(c + 1) * 128], qrot[:, hh, qs_], True, True, [Bkrot[c // 4]] + qB, pSB)
                    for ci in range(2):
                        MM(pS[:, (nl + ci) * 128:(nl + ci + 1) * 128], kctx[:, ci * 128:(ci + 1) * 128], qraw[:, hh, qs_], True, True, [Bkctx] + qB, pSB)
                    u = u_ % 3
                    ACT(P1[u][:, 0:512], pS[:, 0:512], AF.Exp, pSB + [BP1[u]], [BP1[u]], scale=0.125)
                    if nl == 5:
                        ACT(P1[u][:, 512:640], pS[:, 512:640], AF.Exp, pSB + [BP1[u]], [BP1[u]], scale=0.125)
                    ACT(Pt[u][:, nl * 128:(nl + 2) * 128], pS[:, nl * 128:(nl + 2) * 128], AF.Exp, pSB + [BPt[u]], [BPt[u]], scale=0.125)
                    eoff = p0 * 128
                    TT("pool" if u_ % 2 == 0 else "dve", Pt[u][:, 0:nl * 128], P1[u][:, 0:nl * 128], Ebh[hh][:, eoff:eoff + nl * 128], ALU.mult, [BP1[u], BEbh[hh], BPt[u]], [BPt[u]])

                def stageB(u_):
                    hh, m = units[u_]
                    hs = slice(64 * hh, 64 * hh + 64)
                    p0, chunks = _pat_of_block(m)
                    nl = len(chunks)
                    qs_ = slice(m * 128, (m + 1) * 128)
                    u = u_ % 3
                    bi = 6 + (u_ % 2)
                    po, pOB = PS[3][:, (bi - 6) * 512:(bi - 6) * 512 + 512], PB[bi]
                    nch = nl + 2
                    for ci in range(nch):
                        if ci < nl:
                            c = chunks[ci]
                            vl = Vt[:, c, :]
                            vB = BVt[c // 4]
                        else:
                            vl = vctx[:, ci - nl, :]
                            vB = Bvctx
                        MM(po[:, 0:128], vl, Pt[u][:, ci * 128:(ci + 1) * 128], ci == 0, ci == nch - 1, [vB, BPt[u]], [pOB])
                    for ci in range(nch):
                        MM(po[:, 128:256], ones128[:], Pt[u][:, ci * 128:(ci + 1) * 128], ci == 0, ci == nch - 1, [Bon128, BPt[u]], [pOB])
                    r_ = u_ % 2
                    if u_ % 2 == 0:
                        RECIP(rec[r_][hs, :], po[hs, 128:256], [pOB, Brec[r_]], [Brec[r_]])
                    else:
                        ACT(rec[r_][hs, :], po[hs, 128:256], AF.Ln, [pOB, Brec[r_]], [Brec[r_]])
                        ACT(rec[r_][hs, :], rec[r_][hs, :], AF.Exp, [Brec[r_]], [Brec[r_]], scale=-1.0)
                    TT("dve", mixT[hs, j, qs_], po[hs, 0:128], rec[r_][hs, :], ALU.mult, [pOB, Brec[r_], mixB[j][m // 4]], [mixB[j][m // 4]])

                for u_ in range(len(units) + SKN):
                    if u_ < len(units):
                        stageA(u_)
                    if u_ >= SKN:
                        stageB(u_ - SKN)
                    if u_ == 15 + SKN:
                        prepE(j + 1, 0)
                prepE(j + 1, 1)
            if b == 0:
                dump("mixNA", mixT[:, 0:4, 0:512], [mixB[c_][0] for c_ in range(4)])
            M.release(ropeC, ropeS, maskE, qrot, krot, qraw, Vt, kctx, vctx, *Ebh, *t1, *t2, *P1, *Pt, *rec)
            phase_end("na")
            if stop == "na":
                break

            qs = M.alloc([128, SEQ], F32, "qs")
            sg = M.alloc([128, SEQ], BF16, "sg")
            vtm = M.alloc([64, 32, 128], BF16, "vtm"); Bvtm = [Buf() for _ in range(8)]
            vctm = M.alloc([128, 2, 128], BF16, "vctm"); Bvctm = Buf()
            oT = M.alloc([128, SEQ], F32, "oT"); BoT = [Buf() for _ in range(4)]
            fbuf = M.alloc([128, SEQ], F32, "fbuf")
            Bext = M.alloc([128, SEQ + 64], F32, "Bext")
            k32 = M.alloc([128, SEQ], F32, "k32")
            EX = M.alloc([128, SEQ], F32, "EX")
            qe = M.alloc([128, SEQ], BF16, "qe")
            ke = M.alloc([128, SEQ], BF16, "ke")
            kd = M.alloc([128, SEQ], BF16, "kd")
            dec = M.alloc([128, 32], F32, "dec")
            edl = M.alloc([128, 32], F32, "edl")
            cf = M.alloc([128, LC + 64], F32, "cf"); Bcf = Buf()
            cB = M.alloc([128, LC + 64], F32, "cB"); BcB = Buf()
            ck = M.alloc([128, LC], F32, "ck"); Bck = Buf()
            ckd = M.alloc([128, LC], BF16, "ckd"); Bckd = Buf()
            ckdT = M.alloc([128, 2, 128], BF16, "ckdT"); BckdT = Buf()
            kdT8 = [M.alloc([64, 4, 128], BF16, "kdT8") for _ in range(2)]; BkdT8 = [Buf(), Buf()]
            Atall = M.alloc([64, 32, 64], BF16, "Atall"); BAtq = [Buf() for _ in range(4)]
            S32all = M.alloc([128, 33, 128], F32, "S32all"); BSq = [Buf() for _ in range(5)]
            Sbfall = M.alloc([128, 32, 128], BF16, "Sbfall"); BSbq = [Buf() for _ in range(4)]
            qe2 = M.alloc([128, SEQ], BF16, "qe2")
            S0b = M.alloc([128, 128], F32, "S0b"); BS0b = Buf()

            def chunkv(ap2d, off, n=32):
                return ap2d[:, off:off + n * 64].rearrange("p (c s) -> p c s", s=64)

            Bfb = [Buf() for _ in range(4)]; BEX = [Buf() for _ in range(4)]; Bk32 = [Buf() for _ in range(4)]
            BBx = [Buf() for _ in range(5)]
            Bqe = [Buf() for _ in range(4)]; Bke = [Buf() for _ in range(4)]; Bkd = [Buf() for _ in range(4)]
            Bqe2 = [Buf() for _ in range(4)]; Bdec = [Buf() for _ in range(4)]; Bedl = [Buf() for _ in range(4)]
            Bqs = [Buf() for _ in range(4)]; Bsg = [Buf() for _ in range(4)]
            MSET("dve", Bext[:, 0:1], 0.0, [BBx[4]])
            MSET("dve", cB[:, 0:1], 0.0, [BcB])

            def cv(t, off, tb):
                return t[:, off + tb * 512:off + (tb + 1) * 512].rearrange("p (c s) -> p c s", s=64)

            def make_head(g):
                w, Bw = wg[(4 + g) % 2], Bwg[(4 + g) % 2]

                def proj_gates(dr):
                    c0 = 256 + dr * 128
                    for tb in range(4):
                        hr = hTB[4 * tb:4 * tb + 4]
                        tsl = slice(tb * 512, (tb + 1) * 512)
                        pf, pFB = bank()
                        for k in range(8):
                            MM(pf, w[:, k, c0:c0 + 128], hT[:, k, tsl], k == 0, k == 7, hr + [Bw], [pFB])
                        ACT(fbuf[:, tsl], pf, AF.Sigmoid, [pFB, Bfb[tb]], [Bfb[tb]])
                    pcf, pCFB = bank()
                    for k in range(8):
                        MM(pcf[:, 0:LC], w[:, k, c0:c0 + 128], hcT[:, k, :], k == 0, k == 7, hcTB + [Bw], [pCFB])
                    ACT(cf[:, 0:LC], pcf[:, 0:LC], AF.Sigmoid, [pCFB, Bcf], [Bcf])

                def proj_vg():
                    for q4 in range(8):
                        pv, pVB = bank()
                        for cc in range(4):
                            c = q4 * 4 + cc
                            for k in range(8):
                                MM(pv[0:64, cc * 128:(cc + 1) * 128], hT[:, k, c * 64:(c + 1) * 64], w[:, k, 128:256], k == 0, k == 7, [hTB[c // 2], Bw], [pVB])
                        ACT(vtm[:, q4 * 4:q4 * 4 + 4, :], pv[0:64, :].rearrange("p (t c) -> p t c", c=128), AF.Copy, [pVB, Bvtm[q4]], [Bvtm[q4]])
                    for tb in range(4):
                        hr = hTB[4 * tb:4 * tb + 4]
                        tsl = slice(tb * 512, (tb + 1) * 512)
                        pg, pGB = bank()
                        for k in range(8):
                            MM(pg, w[:, k, 512:640], hT[:, k, tsl], k == 0, k == 7, hr + [Bw], [pGB])
                        ACT(sg[:, tsl], pg, AF.Silu, [pGB, Bsg[tb]], [Bsg[tb]])

                def proj_q():
                    for tb in range(4):
                        hr = hTB[4 * tb:4 * tb + 4]
                        tsl = slice(tb * 512, (tb + 1) * 512)
                        pq, pQB = bank()
                        for k in range(8):
                            MM(pq, w[:, k, 0:128], hT[:, k, tsl], k == 0, k == 7, hr + [Bw], [pQB])
                        ACT(qs[:, tsl], pq, AF.Silu, [pQB, Bqs[tb]], [Bqs[tb]])

                def proj_ctxv():
                    pc, pCB = bank()
                    for ti in range(2):
                        for k in range(8):
                            MM(pc[:, ti * 128:(ti + 1) * 128], hcT[:, k, ti * 128:(ti + 1) * 128], w[:, k, 128:256], k == 0, k == 7, hcTB + [Bw], [pCB])
                    ACT(vctm[:], pc[:, 0:256].rearrange("p (t c) -> p t c", c=128), AF.Copy, [pCB, Bvctm], [Bvctm])

                def prep_early(dr):
                    nonlocal nxt
                    lcol = g * 2 + dr
                    proj_gates(dr)
                    if dr == 1 and g < 3:
                        nxt = load_wg(4 + g + 1)
                    T4 = range(4)
                    tsl_ = lambda tb: slice(tb * 512, (tb + 1) * 512)
                    for tb in T4:
                        TS("dve", fbuf[:, tsl_(tb)], fbuf[:, tsl_(tb)], oml[:, lcol:lcol + 1], lbv[:, lcol:lcol + 1], ALU.mult, ALU.add, [Bfb[tb], Boml, Blbv], [Bfb[tb]])
                    TS("dve", cf[:, 0:LC], cf[:, 0:LC], oml[:, lcol:lcol + 1], lbv[:, lcol:lcol + 1], ALU.mult, ALU.add, [Bcf, Boml, Blbv], [Bcf])
                    for tb in T4:
                        TS("pool", k32[:, tsl_(tb)], fbuf[:, tsl_(tb)], -1.0, 1.0, ALU.mult, ALU.add, [Bfb[tb], Bk32[tb]], [Bk32[tb]])
                    TS("pool", ck[:], cf[:, 0:LC], -1.0, 1.0, ALU.mult, ALU.add, [Bcf, Bck], [Bck])
                    for tb in T4:
                        ACT(fbuf[:, tsl_(tb)], fbuf[:, tsl_(tb)], AF.Ln, [Bfb[tb]], [Bfb[tb]])
                    ACT(cf[:, 0:LC], cf[:, 0:LC], AF.Ln, [Bcf], [Bcf])
                    for tb in T4:
                        MSET("pool", EX[:, tsl_(tb)], 1.0, [BEX[tb]])
                    for tb in T4:
                        prevB = BBx[4] if tb == 0 else BBx[tb - 1]
                        S.op("dve", (lambda tb: lambda e: e.tensor_tensor_scan(Bext[:, 1 + tb * 512:1 + (tb + 1) * 512], EX[:, tb * 512:(tb + 1) * 512],
                                                                                 fbuf[:, tb * 512:(tb + 1) * 512], Bext[:, tb * 512:tb * 512 + 1], ALU.mult, ALU.add))(tb),
                             [BEX[tb], Bfb[tb], prevB, BBx[tb]], [BBx[tb]])
                    S.op("dve", lambda e: e.tensor_tensor_scan(cB[:, 1:LC + 1], EX[:, 0:LC], cf[:, 0:LC], 0.0, ALU.mult, ALU.add), [BEX[0], Bcf, BcB], [BcB])
                    if dr == 0:
                        TT("dve", cf[:, 0:LC], cB[:, 1:LC + 1], cB[:, LC:LC + 1].to_broadcast([128, LC]), ALU.subtract, [BcB, Bcf], [Bcf])
                        ACT(cf[:, 0:LC], cf[:, 0:LC], AF.Exp, [Bcf], [Bcf], scale=-1.0)
                    else:
                        ACT(cf[:, 0:LC], cB[:, 0:LC], AF.Exp, [BcB, Bcf], [Bcf])
                    TT("dve", ckd[:], ck[:], cf[:, 0:LC], ALU.mult, [Bck, Bcf, Bckd], [Bckd])
                    if dr == 0:
                        proj_q()
                        proj_ctxv()
                    pt, pTB = bank()
                    for ti in range(2):
                        MM(pt[:, ti * 128:(ti + 1) * 128], ckd[:, ti * 128:(ti + 1) * 128], ident16[:], True, True, [Bckd, Bid16], [pTB])
                    ACT(ckdT[:], pt[:, 0:256].rearrange("p (t c) -> p t c", c=128), AF.Copy, [pTB, BckdT], [BckdT])
                    pz, pZB = bank()
                    for ti in range(2):
                        MM(pz[:, 0:128], ckdT[:, ti, :], vctm[:, ti, :], ti == 0, ti == 1, [BckdT, Bvctm], [pZB])
                    CP("dve", S0b[:], pz[:, 0:128], [pZB, BS0b], [BS0b])
                def prep_late(dr):
                    T4 = range(4)
                    tsl_ = lambda tb: slice(tb * 512, (tb + 1) * 512)
                    CP("dve", S32all[:, 0, :], S0b[:], [BS0b, BSq[0]], [BSq[0]])
                    toff = 1 if dr == 0 else 0
                    bxr = lambda tb: [BBx[tb], BBx[4] if tb == 0 else BBx[tb - 1]]
                    bc8 = lambda v: v.to_broadcast([128, 8, 64])
                    for tb in T4:
                        TT("dve", cv(fbuf, 0, tb), cv(Bext, toff, tb), bc8(cv(Bext, 32, tb)[:, :, 0:1]), ALU.subtract, bxr(tb) + [Bfb[tb]], [Bfb[tb]])
                    for tb in T4:
                        ACT(EX[:, tsl_(tb)], fbuf[:, tsl_(tb)], AF.Exp, [Bfb[tb], BEX[tb]], [BEX[tb]], scale=(1.0 if dr == 0 else -1.0))
                    for tb in T4:
                        TT("dve", qe[:, tsl_(tb)], qs[:, tsl_(tb)], EX[:, tsl_(tb)], ALU.mult, [Bqs[tb], BEX[tb], Bqe[tb]], [Bqe[tb]])
                    for tb in T4:
                        ACT(EX[:, tsl_(tb)], fbuf[:, tsl_(tb)], AF.Exp, [Bfb[tb], BEX[tb]], [BEX[tb]], scale=(-1.0 if dr == 0 else 1.0))
                    for tb in T4:
                        TT("dve", ke[:, tsl_(tb)], k32[:, tsl_(tb)], EX[:, tsl_(tb)], ALU.mult, [Bk32[tb], BEX[tb], Bke[tb]], [Bke[tb]])
                    for tb in T4:
                        ref_ = cv(Bext, 64, tb)[:, :, 0:1] if dr == 0 else cv(Bext, 0, tb)[:, :, 0:1]
                        TT("dve", cv(fbuf, 0, tb), cv(Bext, toff, tb), bc8(ref_), ALU.subtract, bxr(tb) + [Bfb[tb]], [Bfb[tb]])
                    for tb in T4:
                        ACT(EX[:, tsl_(tb)], fbuf[:, tsl_(tb)], AF.Exp, [Bfb[tb], BEX[tb]], [BEX[tb]], scale=(-1.0 if dr == 0 else 1.0))
                    for tb in T4:
                        TT("pool", kd[:, tsl_(tb)], k32[:, tsl_(tb)], EX[:, tsl_(tb)], ALU.mult, [Bk32[tb], BEX[tb], Bkd[tb]], [Bkd[tb]])
                    for tb in T4:
                        c8 = slice(8 * tb, 8 * tb + 8)
                        end_ = cv(Bext, 64, tb)[:, :, 0]
                        beg_ = cv(Bext, 0, tb)[:, :, 0]
                        mid_ = cv(Bext, 32, tb)[:, :, 0]
                        TT("dve", dec[:, c8], end_, beg_, ALU.subtract, bxr(tb) + [Bdec[tb]], [Bdec[tb]])
                        if dr == 0:
                            TT("dve", edl[:, c8], mid_, beg_, ALU.subtract, bxr(tb) + [Bedl[tb]], [Bedl[tb]])
                        else:
                            TT("dve", edl[:, c8], end_, mid_, ALU.subtract, bxr(tb) + [Bedl[tb]], [Bedl[tb]])
                    for tb in T4:
                        c8 = slice(8 * tb, 8 * tb + 8)
                        ACT(dec[:, c8], dec[:, c8], AF.Exp, [Bdec[tb]], [Bdec[tb]])
                        ACT(edl[:, c8], edl[:, c8], AF.Exp, [Bedl[tb]], [Bedl[tb]])
                    for tb in T4:
                        c8 = slice(8 * tb, 8 * tb + 8)
                        TT("pool", cv(qe2, 0, tb), cv(qe, 0, tb), edl[:, c8].rearrange("p (c o) -> p c o", o=1).to_broadcast([128, 8, 64]), ALU.mult,
                           [Bqe[tb], Bedl[tb], Bqe2[tb]], [Bqe2[tb]])
                def passes(dr):
                    order = list(range(32)) if dr == 0 else list(range(31, -1, -1))
                    msk = tri[:, 0:64] if dr == 0 else tri[:, 64:128]
                    SK = 2

                    def supd(i):
                        c_ = order[i]
                        r4_ = i % 4
                        ps_, pSB_ = bank()
                        MM(ps_[:, 0:128], kdT8[(i // 4) % 2][:, i % 4, :], vtm[:, c_, :], True, True, [BkdT8[(i // 4) % 2], Bvtm[c_ // 4]], [pSB_])
                        STT("dve", S32all[:, i + 1, :], S32all[:, i, :], dec[:, c_:c_ + 1], ps_[:, 0:128], ALU.mult, ALU.add,
                            [BSq[i // 8], Bdec[c_ // 8], pSB_, BSq[(i + 1) // 8]], [BSq[(i + 1) // 8]])
                        if i % 8 == 6:
                            q_ = i // 8
                            ACT(Sbfall[:, 8 * q_:8 * q_ + 8, :], S32all[:, 8 * q_:8 * q_ + 8, :], AF.Copy, [BSq[q_], BSbq[q_]], [BSbq[q_]])

                    GP = 4
                    for g0 in range(0, 32, GP):
                        pxa, pXA = bank()
                        pxk, pXK = bank()
                        for q_ in range(GP):
                            idx = g0 + q_
                            c = order[idx]
                            cs_ = slice(c * 64, (c + 1) * 64)
                            pos_ = q_ if dr == 0 else GP - 1 - q_
                            MM(pxa[0:64, pos_ * 64:(pos_ + 1) * 64], ke[:, cs_], qe[:, cs_], True, True, [Bke[c // 8], Bqe[c // 8]], [pXA])
                        for q_ in range(GP):
                            idx = g0 + q_
                            c = order[idx]
                            cs_ = slice(c * 64, (c + 1) * 64)
                            MM(pxk[0:64, q_ * 128:(q_ + 1) * 128], kd[:, cs_], ident16[:], True, True, [Bkd[c // 8], Bid16], [pXK])
                        cmin = min(order[g0], order[g0 + GP - 1])
                        TT("dve", Atall[:, cmin:cmin + GP, :], pxa[0:64, 0:GP * 64].rearrange("p (q s) -> p q s", s=64),
                           msk.rearrange("p (o s) -> p o s", o=1).to_broadcast([64, GP, 64]), ALU.mult, [pXA, Btri, BAtq[cmin // 8]], [BAtq[cmin // 8]])
                        sl8 = (g0 // GP) % 2
                        ACT(kdT8[sl8][:], pxk[0:64, :].rearrange("p (q d) -> p q d", d=128), AF.Copy, [pXK, BkdT8[sl8]], [BkdT8[sl8]])
                        if g0 >= GP:
                            for q_ in range(GP):
                                supd(g0 - GP + q_)
                    for i in range(32 - GP, 31):
                        supd(i)
                    for g0 in range(0, 32, 8):
                        po, pOB = bank()
                        for q_ in range(8):
                            idx = g0 + q_
                            c = order[idx]
                            oc = (c % 8) * 64
                            MM(po[:, oc:oc + 64], Sbfall[:, idx, :], qe2[:, c * 64:(c + 1) * 64], True, False, [BSbq[idx // 8], Bqe2[c // 8]], [pOB])
                            MM(po[:, oc:oc + 64], vtm[:, c, :], Atall[:, c, :], False, True, [Bvtm[c // 4], BAtq[c // 8]], [pOB])
                        if True:
                            tb = order[g0] // 8
                            if dr == 0:
                                ACT(oT[:, tb * 512:(tb + 1) * 512], po, AF.Copy, [pOB, BoT[tb]], [BoT[tb]])
                            else:
                                TT("dve", oT[:, tb * 512:(tb + 1) * 512], oT[:, tb * 512:(tb + 1) * 512], po, ALU.add, [pOB, BoT[tb]], [BoT[tb]])
                def outnorm():
                    T4_ = range(4)
                    ts2 = lambda tb: slice(tb * 512, (tb + 1) * 512)
                    for tb in T4_:
                        ACT(qe[:, ts2(tb)], oT[:, ts2(tb)], AF.Square, [BoT[tb], Bqe[tb]], [Bqe[tb]])
                    pms = []
                    for tb in T4_:
                        pm, pMB = bank()
                        MM(pm, onesm16[:], qe[:, ts2(tb)], True, True, [Bonm16, Bqe[tb]], [pMB])
                        pms.append((pm, pMB))
                    for tb in T4_:
                        pm, pMB = pms[tb]
                        ACT(fbuf[:, ts2(tb)], pm, AF.Ln, [pMB, Bfb[tb], Bepsb], [Bfb[tb]], bias=epsb[:], scale=1.0)
                    for tb in T4_:
                        ACT(fbuf[:, ts2(tb)], fbuf[:, ts2(tb)], AF.Exp, [Bfb[tb]], [Bfb[tb]], scale=-0.5)
                    for tb in T4_:
                        TT("dve", EX[:, ts2(tb)], oT[:, ts2(tb)], fbuf[:, ts2(tb)], ALU.mult, [BoT[tb], Bfb[tb], BEX[tb]], [BEX[tb]])
                        STT("dve", mixT[:, 4 + g, ts2(tb)], EX[:, ts2(tb)], hgn[:, 0:1], sg[:, ts2(tb)], ALU.mult, ALU.mult, [BEX[tb], Bhgn, Bsg[tb], mixB[4 + g][tb]], [mixB[4 + g][tb]])

                return dict(proj_gates=proj_gates, proj_q=proj_q, proj_vg=proj_vg, prep_early=prep_early, prep_late=prep_late, passes=passes, outnorm=outnorm)

            H = [make_head(g_) for g_ in range(4)]
            H[0]["prep_early"](0)
            for g_ in range(4):
                h_ = H[g_]
                h_["prep_late"](0)
                h_["proj_vg"]()
                h_["prep_early"](1)
                h_["passes"](0)
                h_["prep_late"](1)
                if g_ < 3:
                    H[g_ + 1]["prep_early"](0)
                h_["passes"](1)
                h_["outnorm"]()
            if b == 0:
                dump("mixHG", mixT[:, 4:8, 0:512], [mixB[c_][0] for c_ in range(4, 8)])
            M.release(qs, sg, vtm, vctm, oT, fbuf, Bext, k32, EX, qe, ke, kd, dec, edl, cf, cB, ck, ckd, ckdT, *kdT8, Atall, S32all, Sbfall, qe2, S0b, *wg)
            phase_end("hg")
            if stop == "hg":
                break

            xres = M.alloc([128, NT, D], F32, "xres")
            BxT = [[Buf("xr%d_%d" % (i, h)) for h in range(2)] for i in range(NT)]
            wo = M.alloc([128, 8, D], BF16, "wo"); Bwo = Buf()
            garep = M.alloc([128, D], F32, "garep"); Bga = Buf()
            Dg = [M.alloc([128, 128], F32, "Dg") for _ in range(2)]; BDg = [Buf(), Buf()]
            tmp = [M.alloc([128, 512], F32, "tmp") for _ in range(4)]; Btmp = [Buf() for _ in range(4)]
            DMA("pool", wo[:], wout_d.rearrange("(k p) n -> p k n", p=128), Bwo, None)

            def make_garep(base):
                for half in range(2):
                    pg_, pGB_ = bank()
                    for kk in range(4):
                        kc = half * 4 + kk
                        s = kc % 2
                        TS("dve", Dg[s][:], ident32[:], modT[:, base + kc, b:b + 1], None, ALU.mult, None, [Bid32, BmodT, BDg[s]], [BDg[s]])
                        MM(pg_[:, kk * 128:(kk + 1) * 128], ones32[:], Dg[s][:], True, True, [Bon32, BDg[s]], [pGB_])
                    CP("dve", garep[:, half * 512:(half + 1) * 512], pg_, [pGB_, Bga], [Bga])

            make_garep(16)
            tcn = {"t": 0}
            for i in range(NT):
                DMA("sp", xres[:, i, :], x_d[b, i * 128:(i + 1) * 128, :], BxT[i][0], None)
                BxT[i][1].w = dict(BxT[i][0].w)
                for nh in range(2):
                    py, pYB = bank()
                    for kc in range(8):
                        MM(py, mixT[:, kc, i * 128:(i + 1) * 128], wo[:, kc, nh * 512:(nh + 1) * 512], kc == 0, kc == 7, [mixB[kc][i // 4], Bwo], [pYB])
                    s = tcn["t"] % 4
                    tcn["t"] += 1
                    TT("dve", tmp[s][:], py, garep[:, nh * 512:(nh + 1) * 512], ALU.mult, [pYB, Bga, Btmp[s]], [Btmp[s]])
                    xs = xres[:, i, nh * 512:(nh + 1) * 512]
                    TT("pool", xs, xs, tmp[s][:], ALU.add, [Btmp[s], BxT[i][nh]], [BxT[i][nh]])
            if b == 0:
                dump("x1", xres[:, 0:2, :], [BxT[0][0], BxT[0][1], BxT[1][0], BxT[1][1]])
            M.release(mixT, wo, *Dg)
            phase_end("p3")
            if stop == "p3":
                break

            w1s = [M.alloc([128, 8, 512], BF16, "w1s") for _ in range(2)]
            w3s = [M.alloc([128, 8, 512], BF16, "w3s") for _ in range(2)]
            w2s = [M.alloc([128, 4, D], BF16, "w2s") for _ in range(2)]
            Bw1 = [Buf(), Buf()]; Bw3 = [Buf(), Buf()]; Bw2 = [Buf(), Buf()]
            sa = [M.alloc([128, 512], F32, "sa") for _ in range(2)]; Bsa = [Buf(), Buf()]
            uT = [M.alloc([128, 4, 512], BF16, "uT") for _ in range(2)]; BuT = [[Buf() for _ in range(4)] for _ in range(2)]

            def load_exp(e):
                s = e % 2
                DMA("pool", w1s[s][:], w1_d[e].rearrange("(k p) n -> p k n", p=128), Bw1[s], None)
                DMA("pool", w3s[s][:], w3_d[e].rearrange("(k p) n -> p k n", p=128), Bw3[s], None)
                DMA("pool", w2s[s][:], w2_d[e].rearrange("(k p) n -> p k n", p=128), Bw2[s], None)

            load_exp(0)
            junk = M.alloc([128, D], BF16, "junk"); Bjunk = Buf()
            xn = [M.alloc([128, D], F32, "xn") for _ in range(2)]; Bxn = [Buf() for _ in range(2)]
            h32 = [M.alloc([128, 8, 128], F32, "h32") for _ in range(2)]; Bh32 = [[Buf(), Buf()] for _ in range(2)]
            rt = M.alloc([128, NT, 64], F32, "rt"); Brt = Buf()
            Lall = M.alloc([128, NT, 20], F32, "Lall"); BLall = Buf()
            make_garep(40)
            MSET("dve", ssq[:], 0.0, [Bssq])
            def p4_stats(i):
                rms_stats(xres[:, i, :], i, [BxT[i][0], BxT[i][1]])
                sl = i % 2
                TS("dve", xn[sl][:], xres[:, i, :], rstd[:, i:i + 1], None, ALU.mult, None, [BxT[i][0], BxT[i][1], Brc[i], Bxn[sl]], [Bxn[sl]])

            def p4_trans(i):
                hs_ = i % 2
                sl = i % 2
                pp, pBs = pair()
                for k in range(8):
                    TR(pp[:, k * 128:(k + 1) * 128], xn[sl][:, k * 128:(k + 1) * 128], [Bxn[sl]], [pBs[k // 4]])
                hA = h32[hs_][:, 0:4, :]
                ppA = pp[:, 0:512].rearrange("p (k t) -> p k t", t=128)
                TT("dve", hA, ppA, A_f[:, 0:4, b:b + 1].to_broadcast([128, 4, 128]), ALU.mult, [pBs[0], BA_f, Bh32[hs_][0]], [Bh32[hs_][0]])
                TT("dve", hA, hA, modT[:, 24:28, b:b + 1].to_broadcast([128, 4, 128]), ALU.add, [BmodT, Bh32[hs_][0]], [Bh32[hs_][0]])
                for k in range(4, 8):
                    ACT(h32[hs_][:, k, :], pp[:, k * 128:(k + 1) * 128], AF.Identity, [pBs[1], BA_f, BmodT, Bh32[hs_][1]], [Bh32[hs_][1]], bias=modT[:, 24 + k, b:b + 1], scale=A_f[:, k, b:b + 1])
                CP("pool", hT[:, :, i * 128:(i + 1) * 128], h32[hs_][:], Bh32[hs_] + [hTB[i]], [hTB[i]])
                pr, pRB = bank()
                for k in range(8):
                    MM(pr[:, 0:20], h32[hs_][:, k, :], wr32[:, k, :], k == 0, k == 7, Bh32[hs_] + [Bwr], [pRB])
                TT("dve", Lall[:, i, :], pr[:, 0:20], brrep[:], ALU.add, [pRB, Bbr, BLall], [BLall])

            for i in range(NT + 1):
                if i < NT:
                    p4_stats(i)
                if i >= 1:
                    p4_trans(i - 1)
            R_ = [Brt]
            RL = [Brt, BLall]
            f = lambda a, b_: rt[:, :, a:b_]
            bc = lambda a: rt[:, :, a:a + 1].to_broadcast([128, NT, 4])
            Lg = Lall[:, :, 0:4]
            S.op("dve", lambda e: e.reduce_max(rt[:, :, 36], Lg, AX.X), RL, R_)
            TT("dve", f(4, 8), Lg, bc(36), ALU.is_equal, RL, R_)
            TT("dve", f(8, 12), Lg, bc(36), ALU.subtract, RL, R_)
            ACT(f(8, 12), f(8, 12), AF.Exp, R_, R_)
            S.op("dve", lambda e: e.reduce_sum(rt[:, :, 37], rt[:, :, 8:12], AX.X), R_, R_)
            RECIP(f(38, 39), f(37, 38), R_, R_)
            for g_ in range(4):
                Le = Lall[:, :, 4 + 4 * g_:8 + 4 * g_]
                if g_ == 0:
                    TT("dve", f(16, 20), Le, bc(4 + g_), ALU.mult, RL, R_)
                else:
                    TT("dve", f(12, 16), Le, bc(4 + g_), ALU.mult, RL, R_)
                    TT("dve", f(16, 20), f(16, 20), f(12, 16), ALU.add, R_, R_)
            S.op("dve", lambda e: e.reduce_max(rt[:, :, 39], rt[:, :, 16:20], AX.X), R_, R_)
            TT("dve", f(20, 24), f(16, 20), bc(39), ALU.is_equal, R_, R_)
            STT("dve", f(24, 28), f(20, 24), -1.0e30, f(16, 20), ALU.mult, ALU.add, R_, R_)
            S.op("dve", lambda e: e.reduce_max(rt[:, :, 40], rt[:, :, 24:28], AX.X), R_, R_)
            TT("dve", f(28, 32), f(24, 28), bc(40), ALU.is_equal, R_, R_)
            TT("dve", f(41, 42), f(40, 41), f(39, 40), ALU.subtract, R_, R_)
            ACT(f(42, 43), f(41, 42), AF.Exp, R_, R_)
            TS("dve", f(43, 44), f(42, 43), 1.0, None, ALU.add, None, R_, R_)
            RECIP(f(43, 44), f(43, 44), R_, R_)
            TT("dve", f(44, 45), f(43, 44), f(38, 39), ALU.mult, R_, R_)
            TT("dve", f(45, 46), f(44, 45), f(42, 43), ALU.mult, R_, R_)
            TT("dve", f(32, 36), f(20, 24), bc(44), ALU.mult, R_, R_)
            TT("dve", f(12, 16), f(28, 32), bc(45), ALU.mult, R_, R_)
            TT("dve", f(32, 36), f(32, 36), f(12, 16), ALU.add, R_, R_)
            for g_ in range(4):
                TT("dve", gate[:, :, 4 * g_:4 * g_ + 4], f(32, 36), bc(4 + g_), ALU.mult, R_ + BgateT, BgateT)
            if b == 0:
                dump("gate", gate[:], BgateT)
                dump("h2T", hT[:, :, 0:256], [hTB[0], hTB[1]])
            M.release(junk, *xn, *h32, rt, Lall)
            if stop == "p4a":
                phase_end("p4a")
                break
            MARKS.append(("p4a", {n: S.eng[n].cnt for n in S.eng}))

            cn = {"s": 0, "u": 0}

            def ab_group(e, s, tb, jc, us):
                hr = hTB[4 * tb:4 * tb + 4]
                tsl = slice(tb * 512, (tb + 1) * 512)
                pa, pAB = bank()
                pb_, pBB = bank()
                for k in range(8):
                    MM(pa, w1s[s][:, k, jc * 128:(jc + 1) * 128], hT[:, k, tsl], k == 0, k == 7, hr + [Bw1[s]], [pAB])
                for k in range(8):
                    MM(pb_, w3s[s][:, k, jc * 128:(jc + 1) * 128], hT[:, k, tsl], k == 0, k == 7, hr + [Bw3[s]], [pBB])
                ss_ = cn["s"] % 2
                cn["s"] += 1
                ACT(sa[ss_][:], pa, AF.Silu, [pAB, Bsa[ss_]], [Bsa[ss_]])
                TT("dve", uT[us][:, jc, :], sa[ss_][:], pb_, ALU.mult, [Bsa[ss_], pBB, BuT[us][jc]], [BuT[us][jc]])

            def y_group(e, s, tb, ti, us):
                i = 4 * tb + ti
                for nh in range(2):
                    py, pYB = bank()
                    for jc in range(4):
                        MM(py, uT[us][:, jc, ti * 128:(ti + 1) * 128], w2s[s][:, jc, nh * 512:(nh + 1) * 512], jc == 0, jc == 3, BuT[us] + [Bw2[s]], [pYB])
                    ts_ = tcn["t"] % 4
                    tcn["t"] += 1
                    STT("dve", tmp[ts_][:], py, gate[:, i, e:e + 1], garep[:, nh * 512:(nh + 1) * 512], ALU.mult, ALU.mult, [pYB, BgateT[i], Bga, Btmp[ts_]], [Btmp[ts_]])
                    xs = xres[:, i, nh * 512:(nh + 1) * 512]
                    TT("pool", xs, xs, tmp[ts_][:], ALU.add, [Btmp[ts_], BxT[i][nh]], [BxT[i][nh]])

            pend = None
            for e in range(16):
                s = e % 2
                for tb in range(4):
                    us = cn["u"] % 2
                    cn["u"] += 1
                    for jc in range(4):
                        ab_group(e, s, tb, jc, us)
                        if pend is not None:
                            y_group(pend[0], pend[1], pend[2], jc, pend[3])
                    pend = (e, s, tb, us)
                    if tb == 0 and e + 1 < 16:
                        load_exp(e + 1)
            for ti in range(4):
                y_group(pend[0], pend[1], pend[2], ti, pend[3])
            M.release(*w1s, *w3s, *w2s, *sa, *uT)
            phase_end("moe")

            nfrep = M.alloc([128, D], F32, "nfrep"); Bnf = Buf()
            ost = [M.alloc([128, D], F32, "ost") for _ in range(2)]; Bost = [Buf(), Buf()]
            junk = M.alloc([128, D], BF16, "junk"); Bjunk = Buf()
            DMA("sp", nfrep[:], nfin_d.partition_broadcast(128), Bnf, None)
            MSET("dve", ssq[:], 0.0, [Bssq])
            for i in range(NT + 2):
                if i < NT:
                    rms_stats(xres[:, i, :], i, [BxT[i][0], BxT[i][1]])
                if i >= 2:
                    i2 = i - 2
                    o = i2 % 2
                    STT("dve", ost[o][:], xres[:, i2, :], rstd[:, i2:i2 + 1], nfrep[:], ALU.mult, ALU.mult, [BxT[i2][0], BxT[i2][1], Brc[i2], Bnf, Bost[o]], [Bost[o]])
                    DMA("sp", out_d[b, i2 * 128:(i2 + 1) * 128, :], ost[o][:], None, Bost[o])
            M.release(xres, garep, *tmp, nfrep, *ost, junk)
            phase_end("fin")

        S.barrier()
        S.emit()
    return nc


def prep_shared(inp):
    f = lambda a: np.ascontiguousarray(np.asarray(a, dtype=np.float32))
    w_in = f(inp["w_in"])[0]
    perm = _perm64()
    groups = []
    for j in range(4):
        cols = []
        qc = np.arange(128 * j, 128 * j + 128)
        pc = np.concatenate([128 * j + perm, 128 * j + 64 + perm])
        cols += [qc, pc, 512 + qc, 512 + pc, 1024 + qc]
        groups.append(w_in[:, np.concatenate(cols)])
    for g in range(4):
        base = 1536
        c = np.arange(128 * g, 128 * g + 128)
        cols = [base + c, base + 512 + c, base + 1024 + c, base + 1536 + c, base + 2048 + c]
        groups.append(w_in[:, np.concatenate(cols)])
    w_ing = np.ascontiguousarray(np.stack(groups, 0))
    ridx, cidx, mask = _na_patterns()
    rpb = f(inp["na_rpb"])[0]
    bg = rpb[:, ridx, cidx]
    bg = bg.transpose(0, 2, 1, 3).reshape(8, 128, NPAT * 128)
    biasg = np.ascontiguousarray(bg.reshape(4, 2, 128, NPAT * 128).transpose(0, 2, 1, 3).reshape(4, 128, 2 * NPAT * 128))
    maskE = np.ascontiguousarray(mask.transpose(1, 0, 2).reshape(128, NPAT * 128))
    C, Sg = _rope_tables()
    lb = f(inp["hg_lb"])
    lbT = np.ascontiguousarray(lb.reshape(2, 2, 4, 128).transpose(3, 2, 1, 0).reshape(128, 16))
    tri = np.zeros((64, 128), np.float32)
    s = np.arange(64)[:, None]
    t = np.arange(64)[None, :]
    tri[:, 0:64] = (s <= t)
    tri[:, 64:128] = (s >= t)
    T8 = lambda v: np.ascontiguousarray(f(v).reshape(-1, 128).T)
    sh = {
        "w_mod": f(inp["w_mod"])[0],
        "b_modT": T8(inp["b_mod"][0]),
        "b_mod_row": f(inp["b_mod"]).reshape(1, 6 * D),
        "nmixT": T8(inp["norm_mix"][0]),
        "nffnT": T8(inp["norm_ffn"][0]),
        "nfin_row": f(inp["norm_final"]).reshape(1, D),
        "w_ing": w_ing,
        "w_out": f(inp["w_out"])[0],
        "biasg": biasg,
        "maskE": maskE,
        "ropeC": C, "ropeS": Sg,
        "lbT": lbT,
        "hgnT": f(inp["hg_norm"])[0].reshape(128, 1),
        "wr": np.ascontiguousarray(np.concatenate([f(inp["w_grp"])[0], f(inp["w_exp"])[0]], axis=1)),
        "br_row": np.concatenate([f(inp["b_grp"])[0], f(inp["b_exp"])[0]]).reshape(1, 20),
        "w1": f(inp["w1"])[0], "w3": f(inp["w3"])[0], "w2": f(inp["w2"])[0],
        "ident": np.eye(128, dtype=np.float32),
        "tri": tri,
    }
    return sh


def core_inputs(inp, sh, bs):
    f = lambda a: np.ascontiguousarray(np.asarray(a, dtype=np.float32))
    c = f(inp["c"])
    cvec = np.stack([c[b] for b in bs] + [f(inp["c_ctx"])] * (3 - len(bs)), 0)
    if len(bs) == 2:
        cvec = np.stack([c[bs[0]], c[bs[1]], f(inp["c_ctx"])], 0)
    else:
        cvec = np.stack([c[bs[0]], c[bs[0]], f(inp["c_ctx"])], 0)
    cT = np.ascontiguousarray(cvec.reshape(3, 8, 128).transpose(2, 1, 0))
    m = dict(sh)
    m["x"] = f(inp["x"])[bs]
    m["ctx"] = f(inp["ctx"])[bs]
    m["cT"] = cT
    return m


_NC_CACHE = {}


def kernel(**inputs):
    sh = prep_shared(inputs)
    if "nc" not in _NC_CACHE:
        _NC_CACHE["nc"] = build(NB=2)
    nc = _NC_CACHE["nc"]
    in_maps = [core_inputs(inputs, sh, [2 * i, 2 * i + 1]) for i in range(8)]
    res = run_bass_kernel_spmd(nc, in_maps, core_ids=list(range(8)))
    out = np.concatenate([np.asarray(r["out"], dtype=np.float32) for r in res.results], axis=0)
    return out
```

```python
import numpy as np
from contextlib import ExitStack
import concourse.bass as bass
import concourse.mybir as mybir
from concourse.bass_utils import run_bass_kernel_spmd

F32 = mybir.dt.float32
BF16 = mybir.dt.bfloat16
AF = mybir.ActivationFunctionType
ALU = mybir.AluOpType
AX = mybir.AxisListType

D = 1024
SEQ = 2048
LC = 256
NT = SEQ // 128
EPS = 1e-6
NPAT = 21
SB_BASE = 16512
SB_END = 229376


class Src:
    def __init__(self, name, sem, step):
        self.name, self.sem, self.step, self.cnt = name, sem, step, 0


class Buf:
    def __init__(self, name=""):
        self.name = name
        self.w = {}
        self.r = {}
        self.dsem = None
        self.excl = False


class Sched:
    def __init__(self, nc, es):
        self.nc, self.es = nc, es
        self.eng, self.q = {}, {}
        for n in ("pe", "act", "dve", "pool", "sp"):
            self.eng[n] = Src(n, es.enter_context(nc.semaphore("s_" + n)), 1)
            self.q[n] = []
        self.seen = {n: {} for n in self.eng}
        self.dsrcs = []
        self.same_engine_sync = True

    def _waits(self, en, reads, writes):
        deps = {}
        E_ = self.eng[en]
        for b in reads:
            for s, v in b.w.items():
                if deps.get(s, 0) < v:
                    deps[s] = v
            if b.excl:
                for s, v in b.r.items():
                    if s is not E_ and deps.get(s, 0) < v:
                        deps[s] = v
        for b in writes:
            for d in (b.w, b.r):
                for s, v in d.items():
                    if deps.get(s, 0) < v:
                        deps[s] = v
        E = self.eng[en]
        out = []
        for s, v in deps.items():
            if s is E and (en in ("pe", "sp") or not self.same_engine_sync):
                continue
            if self.seen[en].get(s, 0) >= v:
                continue
            self.seen[en][s] = v
            out.append((s.sem, v))
        return out

    def op(self, en, fn, reads=(), writes=()):
        E = self.eng[en]
        waits = self._waits(en, reads, writes)
        E.cnt += 1
        self.q[en].append((waits, fn, (E.sem, 1)))
        for b in reads:
            b.r[E] = E.cnt
        for b in writes:
            b.w = {E: E.cnt}
            b.r = {}

    def dma(self, en, dst_buf, src_buf, fn):
        track = dst_buf if dst_buf is not None else src_buf
        if track.dsem is None:
            sem = self.es.enter_context(self.nc.semaphore("d%d" % len(self.dsrcs)))
            track.dsem = Src("dma_" + track.name, sem, 16)
            self.dsrcs.append(track.dsem)
        Dm = track.dsem
        reads = [src_buf] if src_buf is not None else []
        writes = [dst_buf] if dst_buf is not None else []
        waits = self._waits(en, reads, writes)
        Dm.cnt += 16
        self.q[en].append((waits, fn, (Dm.sem, 16)))
        for b in reads:
            b.r[Dm] = Dm.cnt
        for b in writes:
            b.w = {Dm: Dm.cnt}
            b.r = {}
        return Dm

    def barrier(self):
        srcs = list(self.eng.values()) + self.dsrcs
        for en in self.eng:
            waits = []
            for s in srcs:
                if (s is self.eng[en] and en in ("pe", "sp")) or s.cnt == 0:
                    continue
                if self.seen[en].get(s, 0) >= s.cnt:
                    continue
                self.seen[en][s] = s.cnt
                waits.append((s.sem, s.cnt))
            if waits:
                self.q[en].append((waits, None, None))

    def emit(self):
        nc = self.nc
        with nc.Block() as block:
            def mk(en):
                def body(e):
                    for waits, fn, inc in self.q[en]:
                        for sem, v in waits:
                            e.wait_ge(sem, v)
                        if fn is not None:
                            fn(e).then_inc(inc[0], inc[1])
                return body
            block.tensor(mk("pe"))
            block.scalar(mk("act"))
            block.vector(mk("dve"))
            block.gpsimd(mk("pool"))
            block.sync(mk("sp"))


class Mem:
    def __init__(self, nc):
        self.nc = nc
        self.free = [(SB_BASE, SB_END)]
        self.pending = []
        self.n = 0

    def alloc(self, shape, dt, name="t"):
        per = 1
        for s in shape[1:]:
            per *= s
        nbytes = per * (2 if dt == BF16 else 4)
        nbytes = (nbytes + 63) // 64 * 64
        for i, (a, b) in enumerate(self.free):
            if b - a >= nbytes:
                self.free[i] = (a + nbytes, b)
                self.n += 1
                key = (a, tuple(shape), str(dt))
                self._cache = getattr(self, "_cache", {})
                if key in self._cache:
                    t = self._cache[key]
                else:
                    t = self.nc.alloc_sbuf_tensor_at("%s_%d" % (name, self.n), list(shape), dt, offset=a)
                    self._cache[key] = t
                t_rng = (a, a + nbytes)
                self._rng = getattr(self, "_rng", {})
                self._rng[id(t)] = t_rng
                self._keep = getattr(self, "_keep", [])
                self._keep.append(t)
                return t
        raise RuntimeError("SBUF OOM for %s %s; free=%s" % (name, shape, self.free))

    def release(self, *ts):
        for t in ts:
            self.pending.append(self._rng[id(t)])

    def commit(self):
        fr = self.free + self.pending
        self.pending = []
        fr = sorted([f for f in fr if f[1] > f[0]])
        out = []
        for a, b in fr:
            if out and out[-1][1] == a:
                out[-1] = (out[-1][0], b)
            else:
                out.append((a, b))
        self.free = out


def _na_patterns():
    pats = [(2, [0, 1, 2, 3, 4]), (0, [0, 1, 2, 3]), (1, [0, 1, 2, 3]), (14, [12, 13, 14, 15]), (15, [12, 13, 14, 15])]
    ridx = np.zeros((NPAT, 128, 128), np.int64)
    cidx = np.zeros((NPAT, 128, 128), np.int64)
    mask = np.zeros((NPAT, 128, 128), np.float32)
    kk = np.arange(128)[:, None]
    qq = np.arange(128)[None, :]
    pos = 0
    for m, chunks in pats:
        for c in chunks:
            kr = 2 * c + kk // 64
            kc = kk % 64
            r = 2 * m + qq // 64
            cq = qq % 64
            rs = np.clip(r - 4, 0, 24)
            cs = np.clip(cq - 8, 0, 48)
            valid = (kr >= rs) & (kr < rs + 8) & (kc >= cs) & (kc < cs + 16)
            ridx[pos] = np.clip(kr - r + 7, 0, 14)
            cidx[pos] = np.clip(kc - cq + 15, 0, 30)
            mask[pos] = valid
            pos += 1
    assert pos == NPAT
    return ridx, cidx, mask


def _pat_of_block(m):
    if 2 <= m <= 13:
        return 0, [m - 2, m - 1, m, m + 1, m + 2]
    if m == 0:
        return 5, [0, 1, 2, 3]
    if m == 1:
        return 9, [0, 1, 2, 3]
    if m == 14:
        return 13, [12, 13, 14, 15]
    return 17, [12, 13, 14, 15]


def _rope_tables():
    t = np.arange(SEQ)
    pos = np.stack([t // 64, t % 64], axis=-1).astype(np.float32)
    inv = (10000.0 ** (-np.arange(0, 32, 2, dtype=np.float32) / 32)).astype(np.float32)
    ang = pos[:, :, None] * inv
    cos, sin = np.cos(ang).astype(np.float32), np.sin(ang).astype(np.float32)
    C = np.zeros((64, SEQ), np.float32)
    Sg = np.zeros((64, SEQ), np.float32)
    for d in range(64):
        a, j = d // 32, d % 32
        if j < 16:
            C[d] = cos[:, a, j]
            Sg[d] = -sin[:, a, j]
        else:
            C[d] = cos[:, a, j - 16]
            Sg[d] = sin[:, a, j - 16]
    return np.concatenate([C, C], 0), np.concatenate([Sg, Sg], 0)


def _perm64():
    p = np.zeros(64, np.int64)
    for d in range(64):
        a, j = d // 32, d % 32
        p[d] = a * 32 + (j + 16) % 32
    return p


LIM = None
MARKS = []
SUB = 9


def build(NB=2, dbg=None, stop=None):
    dbg = dbg or []
    nc = bass.Bass("TRN2", target_bir_lowering=False)
    es = ExitStack()
    with es:
        S = Sched(nc, es)
        M = Mem(nc)
        dram = lambda n, s, kind="ExternalInput": nc.dram_tensor(n, list(s), F32, kind=kind).ap()
        x_d = dram("x", [NB, SEQ, D])
        ctx_d = dram("ctx", [NB, LC, D])
        cT_d = dram("cT", [128, 8, 3])
        wmod_d = dram("w_mod", [D, 6 * D])
        bmodT_d = dram("b_modT", [128, 48])
        bmodrow_d = dram("b_mod_row", [1, 6 * D])
        nmixT_d = dram("nmixT", [128, 8])
        nffnT_d = dram("nffnT", [128, 8])
        nfin_d = dram("nfin_row", [1, D])
        wing_d = dram("w_ing", [8, D, 640])
        wout_d = dram("w_out", [D, D])
        biasg_d = dram("biasg", [4, 128, 2 * NPAT * 128])
        maskE_d = dram("maskE", [128, NPAT * 128])
        ropeC_d = dram("ropeC", [128, SEQ])
        ropeS_d = dram("ropeS", [128, SEQ])
        lbT_d = dram("lbT", [128, 16])
        hgn_d = dram("hgnT", [128, 1])
        wr_d = dram("wr", [D, 20])
        br_d = dram("br_row", [1, 20])
        w1_d = dram("w1", [16, D, 512])
        w3_d = dram("w3", [16, D, 512])
        w2_d = dram("w2", [16, 512, D])
        ident_d = dram("ident", [128, 128])
        tri_d = dram("tri", [64, 128])
        out_d = dram("out", [NB, SEQ, D], kind="ExternalOutput")
        dbg_d = {n: dram("dbg_" + n, s, kind="ExternalOutput") for n, s in dbg}

        PS = [es.enter_context(nc.psum_tensor("ps%d" % i, [128, 1024], F32)) for i in range(4)]
        PB = [Buf("psb%d" % i) for i in range(8)]
        for b_ in PB:
            b_.excl = True
        rr = {"b": 0, "p": 0}

        def bank():
            i = rr["b"]
            rr["b"] = (i + 1) % 8
            return PS[i // 2][:, (i % 2) * 512:(i % 2) * 512 + 512], PB[i]

        def pair():
            i = rr["p"]
            rr["p"] = (i + 1) % 4
            return PS[i], [PB[2 * i], PB[2 * i + 1]]

        def MM(out, lhsT, rhs, start, stop, R, W):
            S.op("pe", lambda e: e.matmul(out, lhsT, rhs, start=start, stop=stop), R, W)

        def ACT(out, in_, func, R, W, bias=None, scale=None, accum=None):
            kw = {}
            if bias is not None:
                kw["bias"] = bias
            if scale is not None:
                kw["scale"] = scale
            if accum is not None:
                kw["accum_out"] = accum
            S.op("act", lambda e: e.activation(out, in_, func, **kw), R, W)

        def TT(en, out, in0, in1, op, R, W):
            S.op(en, lambda e: e.tensor_tensor(out, in0, in1, op), R, W)

        def TS(en, out, in0, s1, s2, op0, op1, R, W):
            if s2 is None:
                S.op(en, lambda e: e.tensor_scalar(out, in0, s1, None, op0), R, W)
            else:
                S.op(en, lambda e: e.tensor_scalar(out, in0, s1, s2, op0, op1), R, W)

        def STT(en, out, in0, sc, in1, op0, op1, R, W):
            S.op(en, lambda e: e.scalar_tensor_tensor(out, in0, sc, in1, op0, op1), R, W)

        def CP(en, out, in_, R, W):
            S.op(en, lambda e: e.tensor_copy(out, in_), R, W)

        def MSET(en, ap, val, W):
            S.op(en, lambda e: e.memset(ap, val), [], W)

        def RECIP(out, in_, R, W):
            S.op("dve", lambda e: e.reciprocal(out, in_), R, W)

        def RMAX(out, in_, R, W):
            S.op("dve", lambda e: e.reduce_max(out, in_, AX.X), R, W)

        def DMA(en, out, in_, dstB, srcB):
            return S.dma(en, dstB, srcB, lambda e: e.dma_start(out=out, in_=in_))

        def phase_end(label=""):
            S.barrier()
            M.commit()
            MARKS.append((label, {n: S.eng[n].cnt for n in S.eng}))

        def dump(name, ap, Bs):
            if name in dbg_d:
                if not isinstance(Bs, list):
                    Bs = [Bs]
                S.barrier()
                for B_ in Bs[1:]:
                    B_.r = dict(B_.r)
                Dsrc = DMA("pool", dbg_d[name], ap, None, Bs[0])
                for B_ in Bs[1:]:
                    B_.r[Dsrc] = Dsrc.cnt

        hT = M.alloc([128, 8, SEQ], BF16, "hT")
        hTB = [Buf("hT%d" % i) for i in range(NT)]
        hcT = M.alloc([128, 8, LC], BF16, "hcT")
        hcTB = [Buf("hcT%d" % i) for i in range(2)]
        ident32 = M.alloc([128, 128], F32, "ident32"); Bid32 = Buf()
        ident16 = M.alloc([128, 128], BF16, "ident16"); Bid16 = Buf()
        ones32 = M.alloc([128, 128], F32, "ones32"); Bon32 = Buf()
        onesm = M.alloc([128, 128], F32, "onesm"); Bonm = Buf()
        ones16 = M.alloc([128, 64], BF16, "ones16"); Bon16 = Buf()
        onesm16 = M.alloc([128, 128], BF16, "onesm16"); Bonm16 = Buf()
        ones128 = M.alloc([128, 128], BF16, "ones128"); Bon128 = Buf()
        tri = M.alloc([64, 128], F32, "tri"); Btri = Buf()
        modT = M.alloc([128, 48, 3], F32, "modT"); BmodT = Buf()
        bmodT = M.alloc([128, 48], F32, "bmodT"); BbmodT = Buf()
        nmixT = M.alloc([128, 8], F32, "nmixT"); Bnmix = Buf()
        nffnT = M.alloc([128, 8], F32, "nffnT"); Bnffn = Buf()
        A_a = M.alloc([128, 8, 3], F32, "A_a"); BA_a = Buf()
        A_f = M.alloc([128, 8, 3], F32, "A_f"); BA_f = Buf()
        lbraw = M.alloc([128, 16], F32, "lbraw"); Blbraw = Buf()
        lbv = M.alloc([128, 8], F32, "lbv"); Blbv = Buf()
        oml = M.alloc([128, 8], F32, "oml"); Boml = Buf()
        hgn = M.alloc([128, 1], F32, "hgn"); Bhgn = Buf()
        wr32 = M.alloc([128, 8, 20], F32, "wr32"); Bwr = Buf()
        brrep = M.alloc([128, 20], F32, "brrep"); Bbr = Buf()
        ssq = M.alloc([128, 64], F32, "ssq"); Bssq = Buf()
        rstd = M.alloc([128, 64], F32, "rstd"); Brstd = Buf()
        Bsc = [Buf("ssqc%d" % i) for i in range(64)]
        Brc = [Buf("rstdc%d" % i) for i in range(64)]
        gate = M.alloc([128, NT, 16], F32, "gate"); BgateT = [Buf("gate%d" % i) for i in range(NT)]
        epsb = M.alloc([128, 1], F32, "epsb"); Bepsb = Buf()

        DMA("sp", ident32[:], ident_d, Bid32, None)
        DMA("pool", ident16[:], ident_d, Bid16, None)
        DMA("sp", tri[:], tri_d, Btri, None)
        DMA("sp", bmodT[:], bmodT_d, BbmodT, None)
        DMA("sp", nmixT[:], nmixT_d, Bnmix, None)
        DMA("sp", nffnT[:], nffnT_d, Bnffn, None)
        DMA("sp", lbraw[:], lbT_d, Blbraw, None)
        DMA("sp", hgn[:], hgn_d, Bhgn, None)
        DMA("sp", wr32[:], wr_d.rearrange("(k p) n -> p k n", p=128), Bwr, None)
        DMA("sp", brrep[:], br_d.partition_broadcast(128), Bbr, None)
        MSET("dve", ones32[:], 1.0, [Bon32])
        MSET("dve", onesm[:], 1.0 / 128.0, [Bonm])
        MSET("dve", ones16[:], 1.0, [Bon16])
        MSET("dve", onesm16[:], 1.0 / 128.0, [Bonm16])
        MSET("dve", ones128[:], 1.0, [Bon128])
        MSET("dve", epsb[:], EPS, [Bepsb])
        lbr = lbraw[:].rearrange("p (a l) -> p a l", l=2)
        TT("dve", lbv[:], lbr[:, :, 0], lbr[:, :, 1], ALU.subtract, [Blbraw], [Blbv])
        ACT(lbv[:], lbv[:], AF.Sigmoid, [Blbv], [Blbv])
        TS("dve", oml[:], lbv[:], -1.0, 1.0, ALU.mult, ALU.add, [Blbv], [Boml])

        scT = M.alloc([128, 8, 3], F32, "scT"); BscT = Buf()
        wst = [M.alloc([128, 8, 1024], F32, "wst") for _ in range(2)]
        Bwst = [Buf("wst0"), Buf("wst1")]
        DMA("sp", scT[:], cT_d, BscT, None)
        ACT(scT[:], scT[:], AF.Silu, [BscT], [BscT])
        brow = M.alloc([3, 6 * D], F32, "brow"); Bbrow = Buf()
        modrow = M.alloc([3, 6 * D], F32, "modrow"); Bmodrow = [Buf() for _ in range(12)]
        DMA("sp", brow[:], bmodrow_d.partition_broadcast(3), Bbrow, None)
        for j in range(6):
            sl = j % 2
            DMA("sp" if j % 2 == 0 else "act", wst[sl][:], wmod_d[:, j * 1024:(j + 1) * 1024].rearrange("(k p) n -> p k n", p=128), Bwst[sl], None)
            for half in range(2):
                pb, pB = bank()
                col0 = j * 1024 + half * 512
                for k in range(8):
                    MM(pb[0:3, :], scT[:, k, :], wst[sl][:, k, half * 512:(half + 1) * 512], k == 0, k == 7, [Bwst[sl], BscT], [pB])
                TT("dve", modrow[0:3, col0:col0 + 512], pb[0:3, :], brow[0:3, col0:col0 + 512], ALU.add, [pB, Bbrow, Bmodrow[2 * j + half]], [Bmodrow[2 * j + half]])
        pT, pTB_ = bank()
        for ch in range(48):
            MM(pT[:, ch * 3:(ch + 1) * 3], modrow[0:3, ch * 128:(ch + 1) * 128], ident32[0:3, 0:3], True, True, [Bmodrow[ch // 4], Bid32], [pTB_])
        CP("dve", modT[:].rearrange("p c n -> p (c n)"), pT[:, 0:144], [pTB_, BmodT], [BmodT])
        for n in range(3):
            STT("dve", A_a[:, :, n], modT[:, 8:16, n], 1.0, nmixT[:], ALU.add, ALU.mult, [BmodT, Bnmix, BA_a], [BA_a])
            STT("dve", A_f[:, :, n], modT[:, 32:40, n], 1.0, nffnT[:], ALU.add, ALU.mult, [BmodT, Bnffn, BA_f], [BA_f])
        dump("modT", modT[:], BmodT)
        M.release(scT, *wst, brow, modrow)
        phase_end("p0")

        def rms_stats(src_ap, col, R):
            ACT(junk[:], src_ap, AF.Square, R + [Bssq, Bsc[col]], [Bsc[col]], accum=ssq[:, col:col + 1])
            ACT(rstd[:, col:col + 1], ssq[:, col:col + 1], AF.Sqrt, [Bsc[col], Bepsb, Brc[col]], [Brc[col]], bias=epsb[:], scale=1.0 / D)
            RECIP(rstd[:, col:col + 1], rstd[:, col:col + 1], [Brc[col]], [Brc[col]])

        def make_dm(col):
            sl = col % len(Dm)
            TS("dve", Dm[sl][:], ident32[:], rstd[:, col:col + 1], None, ALU.mult, None, [Bid32, Brc[col], BDm[sl]], [BDm[sl]])

        def TR(out, in_, R, W):
            S.op("pe", lambda e: e.transpose(out, in_, ident32[:]), R + [Bid32], W)

        def norm_transpose(src_ap, srcB, col, Acol, shcol, n, dst, dstB, eng, dst32=None):
            sl = col % len(Dm)
            pp, pBs = pair()
            for k in range(8):
                MM(pp[:, k * 128:(k + 1) * 128], src_ap[:, k * 128:(k + 1) * 128], Dm[sl][:], True, True, [srcB, BDm[sl]], pBs)
            for k in range(8):
                o = dst[k] if dst32 is None else dst32[:, k, :]
                if eng == "dve":
                    TS("dve", o, pp[:, k * 128:(k + 1) * 128], Acol(k, n), shcol(k, n), ALU.mult, ALU.add, pBs + [BA_a, BA_f, BmodT, dstB], [dstB])
                else:
                    ACT(o, pp[:, k * 128:(k + 1) * 128], AF.Identity, pBs + [BA_a, BA_f, BmodT, dstB], [dstB], bias=shcol(k, n), scale=Acol(k, n))

        for b in range(NB):
            mixT = M.alloc([128, 8, SEQ], BF16, "mixT")
            mixB = [[Buf("mix%d_%d" % (c, t)) for t in range(4)] for c in range(8)]
            wg = [M.alloc([128, 8, 640], BF16, "wg") for _ in range(2)]
            Bwg = [Buf("wg0"), Buf("wg1")]
            wgi = [0]

            def load_wg(g):
                sl = wgi[0] % 2
                wgi[0] += 1
                DMA("pool", wg[sl][:], wing_d[g].rearrange("(k p) n -> p k n", p=128), Bwg[sl], None)
                return wg[sl], Bwg[sl]

            ropeC = M.alloc([128, SEQ], F32, "ropeC"); BrC = Buf()
            ropeS = M.alloc([128, SEQ], F32, "ropeS"); BrS = Buf()
            maskE = M.alloc([128, NPAT * 128], BF16, "maskE"); BmE = Buf()
            DMA("sp", ropeC[:], ropeC_d, BrC, None)
            DMA("sp", ropeS[:], ropeS_d, BrS, None)
            DMA("pool", maskE[:], maskE_d, BmE, None)
            qrot = M.alloc([128, 2, SEQ], BF16, "qrot"); Bqrot = [Buf() for _ in range(4)]
            krot = M.alloc([128, SEQ], BF16, "krot"); Bkrot = [Buf() for _ in range(4)]
            qraw = M.alloc([128, 2, SEQ], BF16, "qraw"); Bqraw = [Buf() for _ in range(4)]
            MSET("pool", qrot[:], 0.0, Bqrot)
            MSET("pool", qraw[:], 0.0, Bqraw)
            Vt = M.alloc([128, NT, 128], BF16, "Vt"); BVt = [Buf() for _ in range(4)]
            kctx = M.alloc([128, LC], BF16, "kctx"); Bkctx = Buf()
            vctx = M.alloc([128, 2, 128], BF16, "vctx"); Bvctx = Buf()
            Ebh = [M.alloc([128, NPAT * 128], F32, "Ebh") for _ in range(2)]; BEbh = [Buf(), Buf()]

            def prepE(j_, hh_):
                if j_ >= 4:
                    return
                DMA("sp", Ebh[hh_][:], biasg_d[j_][:, hh_ * NPAT * 128:(hh_ + 1) * NPAT * 128], BEbh[hh_], None)
                ACT(Ebh[hh_][:], Ebh[hh_][:], AF.Exp, [BEbh[hh_]], [BEbh[hh_]])
                for p_ in range(3):
                    sl_ = slice(p_ * 7 * 128, (p_ + 1) * 7 * 128)
                    TT("dve", Ebh[hh_][:, sl_], Ebh[hh_][:, sl_], maskE[:, sl_], ALU.mult, [BEbh[hh_], BmE], [BEbh[hh_]])

            t1 = [M.alloc([128, 512], F32, "t1") for _ in range(2)]; Bt1 = [Buf(), Buf()]
            t2 = [M.alloc([128, 512], F32, "t2") for _ in range(2)]; Bt2 = [Buf(), Buf()]
            P1 = [M.alloc([128, 640], F32, "P1") for _ in range(4)]; BP1 = [Buf() for _ in range(4)]
            Pt = [M.alloc([128, 896], BF16, "Pt") for _ in range(4)]; BPt = [Buf() for _ in range(4)]
            rec = [M.alloc([128, 128], F32, "rec") for _ in range(2)]; Brec = [Buf(), Buf()]
            cnt = {"t": 0, "u": 0}
            nxt = load_wg(0)
            prepE(0, 0)
            prepE(0, 1)
            xst = [M.alloc([128, D], F32, "xst") for _ in range(4)]
            Bxst = [Buf("xst%d" % i) for i in range(4)]
            junk = M.alloc([128, D], BF16, "junk"); Bjunk = Buf()
            Dm = [M.alloc([128, 128], F32, "Dm") for _ in range(3)]
            BDm = [Buf("Dm%d" % i) for i in range(3)]
            MSET("dve", ssq[:], 0.0, [Bssq])

            def p1_stats(i):
                sl = i % 4
                if i < NT:
                    src = x_d[b, i * 128:(i + 1) * 128, :]
                else:
                    src = ctx_d[b, (i - NT) * 128:(i - NT + 1) * 128, :]
                DMA("sp", xst[sl][:], src, Bxst[sl], None)
                rms_stats(xst[sl][:], i, [Bxst[sl]])
                make_dm(i)

            def p1_trans(i):
                sl = i % 4
                if i < NT:
                    n = b
                    dst = [hT[:, k, i * 128:(i + 1) * 128] for k in range(8)]
                    dB = hTB[i]
                else:
                    n = 2
                    dst = [hcT[:, k, (i - NT) * 128:(i - NT + 1) * 128] for k in range(8)]
                    dB = hcTB[i - NT]
                norm_transpose(xst[sl], Bxst[sl], i, lambda k, n: A_a[:, k, n:n + 1], lambda k, n: modT[:, k, n:n + 1], n, dst, dB,
                               "dve" if i % 2 == 0 else "act")

            for i in range(NT + 2 + 2):
                if i < NT + 2:
                    p1_stats(i)
                if i >= 2:
                    p1_trans(i - 2)
            if b == 0:
                dump("hT", hT[:, :, 0:256], [hTB[0], hTB[1]])
                dump("ssq", ssq[:], Bsc[0:18])
                dump("rstd", rstd[:], Brc[0:18])
            M.release(*xst, junk, *Dm)
            if stop == "p1":
                phase_end("p1")
                break
            MARKS.append(("p1", {n: S.eng[n].cnt for n in S.eng}))

            for j in range(4 if LIM is None else 1):
                w, Bw = nxt
                if LIM == 1:
                    break
                for tb in range(4):
                    hr = hTB[4 * tb:4 * tb + 4]
                    tsl = slice(tb * 512, (tb + 1) * 512)
                    for which in range(2):
                        c0 = which * 256
                        pa, pAB = bank()
                        pp_, pPB = bank()
                        for k in range(8):
                            MM(pa, w[:, k, c0:c0 + 128], hT[:, k, tsl], k == 0, k == 7, hr + [Bw], [pAB])
                        for k in range(8):
                            MM(pp_, w[:, k, c0 + 128:c0 + 256], hT[:, k, tsl], k == 0, k == 7, hr + [Bw], [pPB])
                        s = cnt["t"] % 2
                        cnt["t"] += 1
                        TT("dve", t1[s][:], pa, ropeC[:, tsl], ALU.mult, [pAB, BrC, Bt1[s]], [Bt1[s]])
                        TT("dve", t2[s][:], pp_, ropeS[:, tsl], ALU.mult, [pPB, BrS, Bt2[s]], [Bt2[s]])
                        if which == 0:
                            if SUB >= 2:
                                for h2_ in range(2):
                                    hp_ = slice(64 * h2_, 64 * h2_ + 64)
                                    ACT(qraw[hp_, h2_, tsl], pa[hp_, :], AF.Copy, [pAB, Bqraw[tb]], [Bqraw[tb]])
                            if SUB >= 3:
                                for h2_ in range(2):
                                    hp_ = slice(64 * h2_, 64 * h2_ + 64)
                                    TT("pool", qrot[hp_, h2_, tsl], t1[s][hp_, :], t2[s][hp_, :], ALU.add, [Bt1[s], Bt2[s], Bqrot[tb]], [Bqrot[tb]])
                        else:
                            if SUB >= 3:
                                TT("pool", krot[:, tsl], t1[s][:], t2[s][:], ALU.add, [Bt1[s], Bt2[s], Bkrot[tb]], [Bkrot[tb]])
                    if SUB < 4:
                        continue
                    pv, pVB = bank()
                    for ti in range(4):
                        i = 4 * tb + ti
                        for k in range(8):
                            MM(pv[:, ti * 128:(ti + 1) * 128], hT[:, k, i * 128:(i + 1) * 128], w[:, k, 512:640], k == 0, k == 7, [hTB[i], Bw], [pVB])
                    ACT(Vt[:, 4 * tb:4 * tb + 4, :], pv.rearrange("p (t c) -> p t c", c=128), AF.Copy, [pVB, BVt[tb]], [BVt[tb]])
                if SUB < 5:
                    break
                pc, pCB = bank()
                for k in range(8):
                    MM(pc[:, 0:LC], w[:, k, 256:384], hcT[:, k, :], k == 0, k == 7, hcTB + [Bw], [pCB])
                for ti in range(2):
                    for k in range(8):
                        MM(pc[:, 256 + ti * 128:256 + (ti + 1) * 128], hcT[:, k, ti * 128:(ti + 1) * 128], w[:, k, 512:640], k == 0, k == 7, hcTB + [Bw], [pCB])
                ACT(kctx[:], pc[:, 0:LC], AF.Copy, [pCB, Bkctx], [Bkctx])
                ACT(vctx[:], pc[:, 256:512].rearrange("p (t c) -> p t c", c=128), AF.Copy, [pCB, Bvctx], [Bvctx])
                nxt = load_wg(j + 1)
                if LIM == 2:
                    break
                units = [(hh_, m_) for hh_ in range(2 if LIM is None else 1) for m_ in range(16 if LIM is None else LIM - 2)]
                SKN = 3
                ust = {}

                def stageA(u_):
                    hh, m = units[u_]
                    hs = slice(64 * hh, 64 * hh + 64)
                    p0, chunks = _pat_of_block(m)
                    nl = len(chunks)
                    qs_ = slice(m * 128, (m + 1) * 128)
                    qB = [Bqrot[m // 4], Bqraw[m // 4]]
                    pi = u_ % 3
                    pS, pSB = PS[pi], [PB[2 * pi], PB[2 * pi + 1]]
                    for ci, c in enumerate(chunks):
                        MM(pS[:, ci * 128:(ci + 1) * 128], krot[:, c * 128:(c + 1) * 128], qrot[:, hh, qs_], True, True, [Bkrot[c // 4]] + qB, pSB)
                    for ci in range(2):
                        MM(pS[:, (nl + ci) * 128:(nl + ci + 1) * 128], kctx[:, ci * 128:(ci + 1) * 128], qraw[:, hh, qs_], True, True, [Bkctx] + qB, pSB)
                    u = u_ % 4
                    ACT(P1[u][:, 0:512], pS[:, 0:512], AF.Exp, pSB + [BP1[u]], [BP1[u]], scale=0.125)
                    if nl == 5:
                        ACT(P1[u][:, 512:640], pS[:, 512:640], AF.Exp, pSB + [BP1[u]], [BP1[u]], scale=0.125)
                    ACT(Pt[u][:, nl * 128:(nl + 2) * 128], pS[:, nl * 128:(nl + 2) * 128], AF.Exp, pSB + [BPt[u]], [BPt[u]], scale=0.125)
                    eoff = p0 * 128
                    TT("pool" if u_ % 2 == 0 else "dve", Pt[u][:, 0:nl * 128], P1[u][:, 0:nl * 128], Ebh[hh][:, eoff:eoff + nl * 128], ALU.mult, [BP1[u], BEbh[hh], BPt[u]], [BPt[u]])

                def stageB(u_):
                    hh, m = units[u_]
                    hs = slice(64 * hh, 64 * hh + 64)
                    p0, chunks = _pat_of_block(m)
                    nl = len(chunks)
                    qs_ = slice(m * 128, (m + 1) * 128)
                    u = u_ % 4
                    bi = 6 + (u_ % 2)
                    po, pOB = PS[3][:, (bi - 6) * 512:(bi - 6) * 512 + 512], PB[bi]
                    nch = nl + 2
                    for ci in range(nch):
                        if ci < nl:
                            c = chunks[ci]
                            vl = Vt[:, c, :]
                            vB = BVt[c // 4]
                        else:
                            vl = vctx[:, ci - nl, :]
                            vB = Bvctx
                        MM(po[:, 0:128], vl, Pt[u][:, ci * 128:(ci + 1) * 128], ci == 0, ci == nch - 1, [vB, BPt[u]], [pOB])
                    for ci in range(nch):
                        MM(po[:, 128:256], ones128[:], Pt[u][:, ci * 128:(ci + 1) * 128], ci == 0, ci == nch - 1, [Bon128, BPt[u]], [pOB])
                    r_ = u_ % 2
                    if u_ % 2 == 0:
                        RECIP(rec[r_][hs, :], po[hs, 128:256], [pOB, Brec[r_]], [Brec[r_]])
                    else:
                        ACT(rec[r_][hs, :], po[hs, 128:256], AF.Ln, [pOB, Brec[r_]], [Brec[r_]])
                        ACT(rec[r_][hs, :], rec[r_][hs, :], AF.Exp, [Brec[r_]], [Brec[r_]], scale=-1.0)
                    TT("dve", mixT[hs, j, qs_], po[hs, 0:128], rec[r_][hs, :], ALU.mult, [pOB, Brec[r_], mixB[j][m // 4]], [mixB[j][m // 4]])

                for u_ in range(len(units) + SKN):
                    if u_ < len(units):
                        stageA(u_)
                    if u_ >= SKN:
                        stageB(u_ - SKN)
                    if u_ == 15 + SKN:
                        prepE(j + 1, 0)
                prepE(j + 1, 1)
            if b == 0:
                dump("mixNA", mixT[:, 0:4, 0:512], [mixB[c_][0] for c_ in range(4)])
            M.release(ropeC, ropeS, maskE, qrot, krot, qraw, Vt, kctx, vctx, *Ebh, *t1, *t2, *P1, *Pt, *rec)
            phase_end("na")
            if stop == "na":
                break

            qs = M.alloc([128, SEQ], F32, "qs")
            sg = M.alloc([128, SEQ], BF16, "sg")
            vtm = M.alloc([64, 32, 128], BF16, "vtm"); Bvtm = [Buf() for _ in range(8)]
            vctm = M.alloc([128, 2, 128], BF16, "vctm"); Bvctm = Buf()
            oT = M.alloc([128, SEQ], F32, "oT"); BoT = [Buf() for _ in range(4)]
            fbuf = M.alloc([128, SEQ], F32, "fbuf")
            Bext = M.alloc([128, SEQ + 64], F32, "Bext")
            k32 = M.alloc([128, SEQ], F32, "k32")
            EX = M.alloc([128, SEQ], F32, "EX")
            qe = M.alloc([128, SEQ], BF16, "qe")
            ke = M.alloc([128, SEQ], BF16, "ke")
            kd = M.alloc([128, SEQ], BF16, "kd")
            dec = M.alloc([128, 32], F32, "dec")
            edl = M.alloc([128, 32], F32, "edl")
            cf = M.alloc([128, LC + 64], F32, "cf"); Bcf = Buf()
            cB = M.alloc([128, LC + 64], F32, "cB"); BcB = Buf()
            ck = M.alloc([128, LC], F32, "ck"); Bck = Buf()
            ckd = M.alloc([128, LC], BF16, "ckd"); Bckd = Buf()
            ckdT = M.alloc([128, 2, 128], BF16, "ckdT"); BckdT = Buf()
            kdT8 = [M.alloc([64, 4, 128], BF16, "kdT8") for _ in range(2)]; BkdT8 = [Buf(), Buf()]
            Atall = M.alloc([64, 32, 64], BF16, "Atall"); BAtq = [Buf() for _ in range(4)]
            S32all = M.alloc([128, 33, 128], F32, "S32all"); BSq = [Buf() for _ in range(5)]
            Sbfall = M.alloc([128, 32, 128], BF16, "Sbfall"); BSbq = [Buf() for _ in range(4)]
            qe2 = M.alloc([128, SEQ], BF16, "qe2")
            S0b = M.alloc([128, 128], F32, "S0b"); BS0b = Buf()

            def chunkv(ap2d, off, n=32):
                return ap2d[:, off:off + n * 64].rearrange("p (c s) -> p c s", s=64)

            Bfb = [Buf() for _ in range(4)]; BEX = [Buf() for _ in range(4)]; Bk32 = [Buf() for _ in range(4)]
            BBx = [Buf() for _ in range(5)]
            Bqe = [Buf() for _ in range(4)]; Bke = [Buf() for _ in range(4)]; Bkd = [Buf() for _ in range(4)]
            Bqe2 = [Buf() for _ in range(4)]; Bdec = [Buf() for _ in range(4)]; Bedl = [Buf() for _ in range(4)]
            Bqs = [Buf() for _ in range(4)]; Bsg = [Buf() for _ in range(4)]
            MSET("dve", Bext[:, 0:1], 0.0, [BBx[4]])
            MSET("dve", cB[:, 0:1], 0.0, [BcB])

            def cv(t, off, tb):
                return t[:, off + tb * 512:off + (tb + 1) * 512].rearrange("p (c s) -> p c s", s=64)

            def make_head(g):
                w, Bw = wg[(4 + g) % 2], Bwg[(4 + g) % 2]

                def proj_gates(dr):
                    c0 = 256 + dr * 128
                    for tb in range(4):
                        hr = hTB[4 * tb:4 * tb + 4]
                        tsl = slice(tb * 512, (tb + 1) * 512)
                        pf, pFB = bank()
                        for k in range(8):
                            MM(pf, w[:, k, c0:c0 + 128], hT[:, k, tsl], k == 0, k == 7, hr + [Bw], [pFB])
                        ACT(fbuf[:, tsl], pf, AF.Sigmoid, [pFB, Bfb[tb]], [Bfb[tb]])
                    pcf, pCFB = bank()
                    for k in range(8):
                        MM(pcf[:, 0:LC], w[:, k, c0:c0 + 128], hcT[:, k, :], k == 0, k == 7, hcTB + [Bw], [pCFB])
                    ACT(cf[:, 0:LC], pcf[:, 0:LC], AF.Sigmoid, [pCFB, Bcf], [Bcf])

                def proj_vg():
                    for q4 in range(8):
                        pv, pVB = bank()
                        for cc in range(4):
                            c = q4 * 4 + cc
                            for k in range(8):
                                MM(pv[0:64, cc * 128:(cc + 1) * 128], hT[:, k, c * 64:(c + 1) * 64], w[:, k, 128:256], k == 0, k == 7, [hTB[c // 2], Bw], [pVB])
                        ACT(vtm[:, q4 * 4:q4 * 4 + 4, :], pv[0:64, :].rearrange("p (t c) -> p t c", c=128), AF.Copy, [pVB, Bvtm[q4]], [Bvtm[q4]])
                    for tb in range(4):
                        hr = hTB[4 * tb:4 * tb + 4]
                        tsl = slice(tb * 512, (tb + 1) * 512)
                        pg, pGB = bank()
                        for k in range(8):
                            MM(pg, w[:, k, 512:640], hT[:, k, tsl], k == 0, k == 7, hr + [Bw], [pGB])
                        ACT(sg[:, tsl], pg, AF.Silu, [pGB, Bsg[tb]], [Bsg[tb]])

                def proj_q():
                    for tb in range(4):
                        hr = hTB[4 * tb:4 * tb + 4]
                        tsl = slice(tb * 512, (tb + 1) * 512)
                        pq, pQB = bank()
                        for k in range(8):
                            MM(pq, w[:, k, 0:128], hT[:, k, tsl], k == 0, k == 7, hr + [Bw], [pQB])
                        ACT(qs[:, tsl], pq, AF.Silu, [pQB, Bqs[tb]], [Bqs[tb]])

                def proj_ctxv():
                    pc, pCB = bank()
                    for ti in range(2):
                        for k in range(8):
                            MM(pc[:, ti * 128:(ti + 1) * 128], hcT[:, k, ti * 128:(ti + 1) * 128], w[:, k, 128:256], k == 0, k == 7, hcTB + [Bw], [pCB])
                    ACT(vctm[:], pc[:, 0:256].rearrange("p (t c) -> p t c", c=128), AF.Copy, [pCB, Bvctm], [Bvctm])

                def prep_early(dr):
                    nonlocal nxt
                    lcol = g * 2 + dr
                    proj_gates(dr)
                    if dr == 1 and g < 3:
                        nxt = load_wg(4 + g + 1)
                    T4 = range(4)
                    tsl_ = lambda tb: slice(tb * 512, (tb + 1) * 512)
                    for tb in T4:
                        TS("dve", fbuf[:, tsl_(tb)], fbuf[:, tsl_(tb)], oml[:, lcol:lcol + 1], lbv[:, lcol:lcol + 1], ALU.mult, ALU.add, [Bfb[tb], Boml, Blbv], [Bfb[tb]])
                    TS("dve", cf[:, 0:LC], cf[:, 0:LC], oml[:, lcol:lcol + 1], lbv[:, lcol:lcol + 1], ALU.mult, ALU.add, [Bcf, Boml, Blbv], [Bcf])
                    for tb in T4:
                        TS("pool", k32[:, tsl_(tb)], fbuf[:, tsl_(tb)], -1.0, 1.0, ALU.mult, ALU.add, [Bfb[tb], Bk32[tb]], [Bk32[tb]])
                    TS("pool", ck[:], cf[:, 0:LC], -1.0, 1.0, ALU.mult, ALU.add, [Bcf, Bck], [Bck])
                    for tb in T4:
                        ACT(fbuf[:, tsl_(tb)], fbuf[:, tsl_(tb)], AF.Ln, [Bfb[tb]], [Bfb[tb]])
                    ACT(cf[:, 0:LC], cf[:, 0:LC], AF.Ln, [Bcf], [Bcf])
                    for tb in T4:
                        MSET("pool", EX[:, tsl_(tb)], 1.0, [BEX[tb]])
                    for tb in T4:
                        prevB = BBx[4] if tb == 0 else BBx[tb - 1]
                        S.op("dve", (lambda tb: lambda e: e.tensor_tensor_scan(Bext[:, 1 + tb * 512:1 + (tb + 1) * 512], EX[:, tb * 512:(tb + 1) * 512],
                                                                                 fbuf[:, tb * 512:(tb + 1) * 512], Bext[:, tb * 512:tb * 512 + 1], ALU.mult, ALU.add))(tb),
                             [BEX[tb], Bfb[tb], prevB, BBx[tb]], [BBx[tb]])
                    S.op("dve", lambda e: e.tensor_tensor_scan(cB[:, 1:LC + 1], EX[:, 0:LC], cf[:, 0:LC], 0.0, ALU.mult, ALU.add), [BEX[0], Bcf, BcB], [BcB])
                    if dr == 0:
                        TT("dve", cf[:, 0:LC], cB[:, 1:LC + 1], cB[:, LC:LC + 1].to_broadcast([128, LC]), ALU.subtract, [BcB, Bcf], [Bcf])
                        ACT(cf[:, 0:LC], cf[:, 0:LC], AF.Exp, [Bcf], [Bcf], scale=-1.0)
                    else:
                        ACT(cf[:, 0:LC], cB[:, 0:LC], AF.Exp, [BcB, Bcf], [Bcf])
                    TT("dve", ckd[:], ck[:], cf[:, 0:LC], ALU.mult, [Bck, Bcf, Bckd], [Bckd])
                    if dr == 0:
                        proj_q()
                        proj_ctxv()
                    pt, pTB = bank()
                    for ti in range(2):
                        MM(pt[:, ti * 128:(ti + 1) * 128], ckd[:, ti * 128:(ti + 1) * 128], ident16[:], True, True, [Bckd, Bid16], [pTB])
                    ACT(ckdT[:], pt[:, 0:256].rearrange("p (t c) -> p t c", c=128), AF.Copy, [pTB, BckdT], [BckdT])
                    pz, pZB = bank()
                    for ti in range(2):
                        MM(pz[:, 0:128], ckdT[:, ti, :], vctm[:, ti, :], ti == 0, ti == 1, [BckdT, Bvctm], [pZB])
                    CP("dve", S0b[:], pz[:, 0:128], [pZB, BS0b], [BS0b])
                def prep_late(dr):
                    T4 = range(4)
                    tsl_ = lambda tb: slice(tb * 512, (tb + 1) * 512)
                    CP("dve", S32all[:, 0, :], S0b[:], [BS0b, BSq[0]], [BSq[0]])
                    toff = 1 if dr == 0 else 0
                    bxr = lambda tb: [BBx[tb], BBx[4] if tb == 0 else BBx[tb - 1]]
                    bc8 = lambda v: v.to_broadcast([128, 8, 64])
                    for tb in T4:
                        TT("dve", cv(fbuf, 0, tb), cv(Bext, toff, tb), bc8(cv(Bext, 32, tb)[:, :, 0:1]), ALU.subtract, bxr(tb) + [Bfb[tb]], [Bfb[tb]])
                    for tb in T4:
                        ACT(EX[:, tsl_(tb)], fbuf[:, tsl_(tb)], AF.Exp, [Bfb[tb], BEX[tb]], [BEX[tb]], scale=(1.0 if dr == 0 else -1.0))
                    for tb in T4:
                        TT("dve", qe[:, tsl_(tb)], qs[:, tsl_(tb)], EX[:, tsl_(tb)], ALU.mult, [Bqs[tb], BEX[tb], Bqe[tb]], [Bqe[tb]])
                    for tb in T4:
                        ACT(EX[:, tsl_(tb)], fbuf[:, tsl_(tb)], AF.Exp, [Bfb[tb], BEX[tb]], [BEX[tb]], scale=(-1.0 if dr == 0 else 1.0))
                    for tb in T4:
                        TT("dve", ke[:, tsl_(tb)], k32[:, tsl_(tb)], EX[:, tsl_(tb)], ALU.mult, [Bk32[tb], BEX[tb], Bke[tb]], [Bke[tb]])
                    for tb in T4:
                        ref_ = cv(Bext, 64, tb)[:, :, 0:1] if dr == 0 else cv(Bext, 0, tb)[:, :, 0:1]
                        TT("dve", cv(fbuf, 0, tb), cv(Bext, toff, tb), bc8(ref_), ALU.subtract, bxr(tb) + [Bfb[tb]], [Bfb[tb]])
                    for tb in T4:
                        ACT(EX[:, tsl_(tb)], fbuf[:, tsl_(tb)], AF.Exp, [Bfb[tb], BEX[tb]], [BEX[tb]], scale=(-1.0 if dr == 0 else 1.0))
                    for tb in T4:
                        TT("pool", kd[:, tsl_(tb)], k32[:, tsl_(tb)], EX[:, tsl_(tb)], ALU.mult, [Bk32[tb], BEX[tb], Bkd[tb]], [Bkd[tb]])
                    for tb in T4:
                        c8 = slice(8 * tb, 8 * tb + 8)
                        end_ = cv(Bext, 64, tb)[:, :, 0]
                        beg_ = cv(Bext, 0, tb)[:, :, 0]
                        mid_ = cv(Bext, 32, tb)[:, :, 0]
                        TT("dve", dec[:, c8], end_, beg_, ALU.subtract, bxr(tb) + [Bdec[tb]], [Bdec[tb]])
                        if dr == 0:
                            TT("dve", edl[:, c8], mid_, beg_, ALU.subtract, bxr(tb) + [Bedl[tb]], [Bedl[tb]])
                        else:
                            TT("dve", edl[:, c8], end_, mid_, ALU.subtract, bxr(tb) + [Bedl[tb]], [Bedl[tb]])
                    for tb in T4:
                        c8 = slice(8 * tb, 8 * tb + 8)
                        ACT(dec[:, c8], dec[:, c8], AF.Exp, [Bdec[tb]], [Bdec[tb]])
                        ACT(edl[:, c8], edl[:, c8], AF.Exp, [Bedl[tb]], [Bedl[tb]])
                    for tb in T4:
                        c8 = slice(8 * tb, 8 * tb + 8)
                        TT("pool", cv(qe2, 0, tb), cv(qe, 0, tb), edl[:, c8].rearrange("p (c o) -> p c o", o=1).to_broadcast([128, 8, 64]), ALU.mult,
                           [Bqe[tb], Bedl[tb], Bqe2[tb]], [Bqe2[tb]])
                def passes(dr):
                    order = list(range(32)) if dr == 0 else list(range(31, -1, -1))
                    msk = tri[:, 0:64] if dr == 0 else tri[:, 64:128]
                    SK = 2

                    def supd(i):
                        c_ = order[i]
                        r4_ = i % 4
                        ps_, pSB_ = bank()
                        MM(ps_[:, 0:128], kdT8[(i // 4) % 2][:, i % 4, :], vtm[:, c_, :], True, True, [BkdT8[(i // 4) % 2], Bvtm[c_ // 4]], [pSB_])
                        STT("dve", S32all[:, i + 1, :], S32all[:, i, :], dec[:, c_:c_ + 1], ps_[:, 0:128], ALU.mult, ALU.add,
                            [BSq[i // 8], Bdec[c_ // 8], pSB_, BSq[(i + 1) // 8]], [BSq[(i + 1) // 8]])
                        if i % 8 == 6:
                            q_ = i // 8
                            ACT(Sbfall[:, 8 * q_:8 * q_ + 8, :], S32all[:, 8 * q_:8 * q_ + 8, :], AF.Copy, [BSq[q_], BSbq[q_]], [BSbq[q_]])

                    GP = 4
                    for g0 in range(0, 32, GP):
                        pxa, pXA = bank()
                        pxk, pXK = bank()
                        for q_ in range(GP):
                            idx = g0 + q_
                            c = order[idx]
                            cs_ = slice(c * 64, (c + 1) * 64)
                            pos_ = q_ if dr == 0 else GP - 1 - q_
                            MM(pxa[0:64, pos_ * 64:(pos_ + 1) * 64], ke[:, cs_], qe[:, cs_], True, True, [Bke[c // 8], Bqe[c // 8]], [pXA])
                        for q_ in range(GP):
                            idx = g0 + q_
                            c = order[idx]
                            cs_ = slice(c * 64, (c + 1) * 64)
                            MM(pxk[0:64, q_ * 128:(q_ + 1) * 128], kd[:, cs_], ident16[:], True, True, [Bkd[c // 8], Bid16], [pXK])
                        cmin = min(order[g0], order[g0 + GP - 1])
                        TT("dve", Atall[:, cmin:cmin + GP, :], pxa[0:64, 0:GP * 64].rearrange("p (q s) -> p q s", s=64),
                           msk.rearrange("p (o s) -> p o s", o=1).to_broadcast([64, GP, 64]), ALU.mult, [pXA, Btri, BAtq[cmin // 8]], [BAtq[cmin // 8]])
                        sl8 = (g0 // GP) % 2
                        ACT(kdT8[sl8][:], pxk[0:64, :].rearrange("p (q d) -> p q d", d=128), AF.Copy, [pXK, BkdT8[sl8]], [BkdT8[sl8]])
                        if g0 >= GP:
                            for q_ in range(GP):
                                supd(g0 - GP + q_)
                    for i in range(32 - GP, 31):
                        supd(i)
                    for g0 in range(0, 32, 8):
                        po, pOB = bank()
                        for q_ in range(8):
                            idx = g0 + q_
                            c = order[idx]
                            oc = (c % 8) * 64
                            MM(po[:, oc:oc + 64], Sbfall[:, idx, :], qe2[:, c * 64:(c + 1) * 64], True, False, [BSbq[idx // 8], Bqe2[c // 8]], [pOB])
                            MM(po[:, oc:oc + 64], vtm[:, c, :], Atall[:, c, :], False, True, [Bvtm[c // 4], BAtq[c // 8]], [pOB])
                        if True:
                            tb = order[g0] // 8
                            if dr == 0:
                                ACT(oT[:, tb * 512:(tb + 1) * 512], po, AF.Copy, [pOB, BoT[tb]], [BoT[tb]])
                            else:
                                TT("dve", oT[:, tb * 512:(tb + 1) * 512], oT[:, tb * 512:(tb + 1) * 512], po, ALU.add, [pOB, BoT[tb]], [BoT[tb]])
                def outnorm():
                    T4_ = range(4)
                    ts2 = lambda tb: slice(tb * 512, (tb + 1) * 512)
                    for tb in T4_:
                        ACT(qe[:, ts2(tb)], oT[:, ts2(tb)], AF.Square, [BoT[tb], Bqe[tb]], [Bqe[tb]])
                    pms = []
                    for tb in T4_:
                        pm, pMB = bank()
                        MM(pm, onesm16[:], qe[:, ts2(tb)], True, True, [Bonm16, Bqe[tb]], [pMB])
                        pms.append((pm, pMB))
                    for tb in T4_:
                        pm, pMB = pms[tb]
                        ACT(fbuf[:, ts2(tb)], pm, AF.Ln, [pMB, Bfb[tb], Bepsb], [Bfb[tb]], bias=epsb[:], scale=1.0)
                    for tb in T4_:
                        ACT(fbuf[:, ts2(tb)], fbuf[:, ts2(tb)], AF.Exp, [Bfb[tb]], [Bfb[tb]], scale=-0.5)
                    for tb in T4_:
                        TT("dve", EX[:, ts2(tb)], oT[:, ts2(tb)], fbuf[:, ts2(tb)], ALU.mult, [BoT[tb], Bfb[tb], BEX[tb]], [BEX[tb]])
                        STT("dve", mixT[:, 4 + g, ts2(tb)], EX[:, ts2(tb)], hgn[:, 0:1], sg[:, ts2(tb)], ALU.mult, ALU.mult, [BEX[tb], Bhgn, Bsg[tb], mixB[4 + g][tb]], [mixB[4 + g][tb]])

                return dict(proj_gates=proj_gates, proj_q=proj_q, proj_vg=proj_vg, prep_early=prep_early, prep_late=prep_late, passes=passes, outnorm=outnorm)

            H = [make_head(g_) for g_ in range(4)]
            H[0]["prep_early"](0)
            for g_ in range(4):
                h_ = H[g_]
                h_["prep_late"](0)
                h_["proj_vg"]()
                h_["prep_early"](1)
                h_["passes"](0)
                h_["prep_late"](1)
                if g_ < 3:
                    H[g_ + 1]["prep_early"](0)
                h_["passes"](1)
                h_["outnorm"]()
            if b == 0:
                dump("mixHG", mixT[:, 4:8, 0:512], [mixB[c_][0] for c_ in range(4, 8)])
            M.release(qs, sg, vtm, vctm, oT, fbuf, Bext, k32, EX, qe, ke, kd, dec, edl, cf, cB, ck, ckd, ckdT, *kdT8, Atall, S32all, Sbfall, qe2, S0b, *wg)
            phase_end("hg")
            if stop == "hg":
                break

            xres = M.alloc([128, NT, D], F32, "xres")
            BxT = [[Buf("xr%d_%d" % (i, h)) for h in range(2)] for i in range(NT)]
            wo = M.alloc([128, 8, D], BF16, "wo"); Bwo = Buf()
            garep = M.alloc([128, D], F32, "garep"); Bga = Buf()
            Dg = [M.alloc([128, 128], F32, "Dg") for _ in range(2)]; BDg = [Buf(), Buf()]
            tmp = [M.alloc([128, 512], F32, "tmp") for _ in range(4)]; Btmp = [Buf() for _ in range(4)]
            DMA("pool", wo[:], wout_d.rearrange("(k p) n -> p k n", p=128), Bwo, None)

            def make_garep(base):
                for half in range(2):
                    pg_, pGB_ = bank()
                    for kk in range(4):
                        kc = half * 4 + kk
                        s = kc % 2
                        TS("dve", Dg[s][:], ident32[:], modT[:, base + kc, b:b + 1], None, ALU.mult, None, [Bid32, BmodT, BDg[s]], [BDg[s]])
                        MM(pg_[:, kk * 128:(kk + 1) * 128], ones32[:], Dg[s][:], True, True, [Bon32, BDg[s]], [pGB_])
                    CP("dve", garep[:, half * 512:(half + 1) * 512], pg_, [pGB_, Bga], [Bga])

            make_garep(16)
            tcn = {"t": 0}
            for i in range(NT):
                DMA("sp", xres[:, i, :], x_d[b, i * 128:(i + 1) * 128, :], BxT[i][0], None)
                BxT[i][1].w = dict(BxT[i][0].w)
                for nh in range(2):
                    py, pYB = bank()
                    for kc in range(8):
                        MM(py, mixT[:, kc, i * 128:(i + 1) * 128], wo[:, kc, nh * 512:(nh + 1) * 512], kc == 0, kc == 7, [mixB[kc][i // 4], Bwo], [pYB])
                    s = tcn["t"] % 4
                    tcn["t"] += 1
                    TT("dve", tmp[s][:], py, garep[:, nh * 512:(nh + 1) * 512], ALU.mult, [pYB, Bga, Btmp[s]], [Btmp[s]])
                    xs = xres[:, i, nh * 512:(nh + 1) * 512]
                    TT("pool", xs, xs, tmp[s][:], ALU.add, [Btmp[s], BxT[i][nh]], [BxT[i][nh]])
            if b == 0:
                dump("x1", xres[:, 0:2, :], [BxT[0][0], BxT[0][1], BxT[1][0], BxT[1][1]])
            M.release(mixT, wo, *Dg)
            phase_end("p3")
            if stop == "p3":
                break

            w1s = [M.alloc([128, 8, 512], BF16, "w1s") for _ in range(2)]
            w3s = [M.alloc([128, 8, 512], BF16, "w3s") for _ in range(2)]
            w2s = [M.alloc([128, 4, D], BF16, "w2s") for _ in range(2)]
            Bw1 = [Buf(), Buf()]; Bw3 = [Buf(), Buf()]; Bw2 = [Buf(), Buf()]
            sa = [M.alloc([128, 512], F32, "sa") for _ in range(2)]; Bsa = [Buf(), Buf()]
            uT = [M.alloc([128, 4, 512], BF16, "uT") for _ in range(2)]; BuT = [[Buf() for _ in range(4)] for _ in range(2)]

            def load_exp(e):
                s = e % 2
                DMA("pool", w1s[s][:], w1_d[e].rearrange("(k p) n -> p k n", p=128), Bw1[s], None)
                DMA("pool", w3s[s][:], w3_d[e].rearrange("(k p) n -> p k n", p=128), Bw3[s], None)
                DMA("pool", w2s[s][:], w2_d[e].rearrange("(k p) n -> p k n", p=128), Bw2[s], None)

            load_exp(0)
            junk = M.alloc([128, D], BF16, "junk"); Bjunk = Buf()
            xn = [M.alloc([128, D], F32, "xn") for _ in range(2)]; Bxn = [Buf() for _ in range(2)]
            h32 = [M.alloc([128, 8, 128], F32, "h32") for _ in range(2)]; Bh32 = [[Buf(), Buf()] for _ in range(2)]
            rt = M.alloc([128, NT, 64], F32, "rt"); Brt = Buf()
            Lall = M.alloc([128, NT, 20], F32, "Lall"); BLall = Buf()
            make_garep(40)
            MSET("dve", ssq[:], 0.0, [Bssq])
            def p4_stats(i):
                rms_stats(xres[:, i, :], i, [BxT[i][0], BxT[i][1]])
                sl = i % 2
                TS("dve", xn[sl][:], xres[:, i, :], rstd[:, i:i + 1], None, ALU.mult, None, [BxT[i][0], BxT[i][1], Brc[i], Bxn[sl]], [Bxn[sl]])

            def p4_trans(i):
                hs_ = i % 2
                sl = i % 2
                pp, pBs = pair()
                for k in range(8):
                    TR(pp[:, k * 128:(k + 1) * 128], xn[sl][:, k * 128:(k + 1) * 128], [Bxn[sl]], [pBs[k // 4]])
                hA = h32[hs_][:, 0:4, :]
                ppA = pp[:, 0:512].rearrange("p (k t) -> p k t", t=128)
                TT("dve", hA, ppA, A_f[:, 0:4, b:b + 1].to_broadcast([128, 4, 128]), ALU.mult, [pBs[0], BA_f, Bh32[hs_][0]], [Bh32[hs_][0]])
                TT("dve", hA, hA, modT[:, 24:28, b:b + 1].to_broadcast([128, 4, 128]), ALU.add, [BmodT, Bh32[hs_][0]], [Bh32[hs_][0]])
                for k in range(4, 8):
                    ACT(h32[hs_][:, k, :], pp[:, k * 128:(k + 1) * 128], AF.Identity, [pBs[1], BA_f, BmodT, Bh32[hs_][1]], [Bh32[hs_][1]], bias=modT[:, 24 + k, b:b + 1], scale=A_f[:, k, b:b + 1])
                CP("pool", hT[:, :, i * 128:(i + 1) * 128], h32[hs_][:], Bh32[hs_] + [hTB[i]], [hTB[i]])
                pr, pRB = bank()
                for k in range(8):
                    MM(pr[:, 0:20], h32[hs_][:, k, :], wr32[:, k, :], k == 0, k == 7, Bh32[hs_] + [Bwr], [pRB])
                TT("dve", Lall[:, i, :], pr[:, 0:20], brrep[:], ALU.add, [pRB, Bbr, BLall], [BLall])

            for i in range(NT + 1):
                if i < NT:
                    p4_stats(i)
                if i >= 1:
                    p4_trans(i - 1)
            R_ = [Brt]
            RL = [Brt, BLall]
            f = lambda a, b_: rt[:, :, a:b_]
            bc = lambda a: rt[:, :, a:a + 1].to_broadcast([128, NT, 4])
            Lg = Lall[:, :, 0:4]
            S.op("dve", lambda e: e.reduce_max(rt[:, :, 36], Lg, AX.X), RL, R_)
            TT("dve", f(4, 8), Lg, bc(36), ALU.is_equal, RL, R_)
            TT("dve", f(8, 12), Lg, bc(36), ALU.subtract, RL, R_)
            ACT(f(8, 12), f(8, 12), AF.Exp, R_, R_)
            S.op("dve", lambda e: e.reduce_sum(rt[:, :, 37], rt[:, :, 8:12], AX.X), R_, R_)
            RECIP(f(38, 39), f(37, 38), R_, R_)
            for g_ in range(4):
                Le = Lall[:, :, 4 + 4 * g_:8 + 4 * g_]
                if g_ == 0:
                    TT("dve", f(16, 20), Le, bc(4 + g_), ALU.mult, RL, R_)
                else:
                    TT("dve", f(12, 16), Le, bc(4 + g_), ALU.mult, RL, R_)
                    TT("dve", f(16, 20), f(16, 20), f(12, 16), ALU.add, R_, R_)
            S.op("dve", lambda e: e.reduce_max(rt[:, :, 39], rt[:, :, 16:20], AX.X), R_, R_)
            TT("dve", f(20, 24), f(16, 20), bc(39), ALU.is_equal, R_, R_)
            STT("dve", f(24, 28), f(20, 24), -1.0e30, f(16, 20), ALU.mult, ALU.add, R_, R_)
            S.op("dve", lambda e: e.reduce_max(rt[:, :, 40], rt[:, :, 24:28], AX.X), R_, R_)
            TT("dve", f(28, 32), f(24, 28), bc(40), ALU.is_equal, R_, R_)
            TT("dve", f(41, 42), f(40, 41), f(39, 40), ALU.subtract, R_, R_)
            ACT(f(42, 43), f(41, 42), AF.Exp, R_, R_)
            TS("dve", f(43, 44), f(42, 43), 1.0, None, ALU.add, None, R_, R_)
            RECIP(f(43, 44), f(43, 44), R_, R_)
            TT("dve", f(44, 45), f(43, 44), f(38, 39), ALU.mult, R_, R_)
            TT("dve", f(45, 46), f(44, 45), f(42, 43), ALU.mult, R_, R_)
            TT("dve", f(32, 36), f(20, 24), bc(44), ALU.mult, R_, R_)
            TT("dve", f(12, 16), f(28, 32), bc(45), ALU.mult, R_, R_)
            TT("dve", f(32, 36), f(32, 36), f(12, 16), ALU.add, R_, R_)
            for g_ in range(4):
                TT("dve", gate[:, :, 4 * g_:4 * g_ + 4], f(32, 36), bc(4 + g_), ALU.mult, R_ + BgateT, BgateT)
            if b == 0:
                dump("gate", gate[:], BgateT)
                dump("h2T", hT[:, :, 0:256], [hTB[0], hTB[1]])
            M.release(junk, *xn, *h32, rt, Lall)
            if stop == "p4a":
                phase_end("p4a")
                break
            MARKS.append(("p4a", {n: S.eng[n].cnt for n in S.eng}))

            cn = {"s": 0, "u": 0}

            def ab_group(e, s, tb, jc, us):
                hr = hTB[4 * tb:4 * tb + 4]
                tsl = slice(tb * 512, (tb + 1) * 512)
                pa, pAB = bank()
                pb_, pBB = bank()
                for k in range(8):
                    MM(pa, w1s[s][:, k, jc * 128:(jc + 1) * 128], hT[:, k, tsl], k == 0, k == 7, hr + [Bw1[s]], [pAB])
                for k in range(8):
                    MM(pb_, w3s[s][:, k, jc * 128:(jc + 1) * 128], hT[:, k, tsl], k == 0, k == 7, hr + [Bw3[s]], [pBB])
                ss_ = cn["s"] % 2
                cn["s"] += 1
                ACT(sa[ss_][:], pa, AF.Silu, [pAB, Bsa[ss_]], [Bsa[ss_]])
                TT("dve", uT[us][:, jc, :], sa[ss_][:], pb_, ALU.mult, [Bsa[ss_], pBB, BuT[us][jc]], [BuT[us][jc]])

            def y_group(e, s, tb, ti, us):
                i = 4 * tb + ti
                for nh in range(2):
                    py, pYB = bank()
                    for jc in range(4):
                        MM(py, uT[us][:, jc, ti * 128:(ti + 1) * 128], w2s[s][:, jc, nh * 512:(nh + 1) * 512], jc == 0, jc == 3, BuT[us] + [Bw2[s]], [pYB])
                    ts_ = tcn["t"] % 4
                    tcn["t"] += 1
                    STT("dve", tmp[ts_][:], py, gate[:, i, e:e + 1], garep[:, nh * 512:(nh + 1) * 512], ALU.mult, ALU.mult, [pYB, BgateT[i], Bga, Btmp[ts_]], [Btmp[ts_]])
                    xs = xres[:, i, nh * 512:(nh + 1) * 512]
                    TT("pool", xs, xs, tmp[ts_][:], ALU.add, [Btmp[ts_], BxT[i][nh]], [BxT[i][nh]])

            pend = None
            for e in range(16):
                s = e % 2
                for tb in range(4):
                    us = cn["u"] % 2
                    cn["u"] += 1
                    for jc in range(4):
                        ab_group(e, s, tb, jc, us)
                        if pend is not None:
                            y_group(pend[0], pend[1], pend[2], jc, pend[3])
                    pend = (e, s, tb, us)
                    if tb == 0 and e + 1 < 16:
                        load_exp(e + 1)
            for ti in range(4):
                y_group(pend[0], pend[1], pend[2], ti, pend[3])
            M.release(*w1s, *w3s, *w2s, *sa, *uT)
            phase_end("moe")

            nfrep = M.alloc([128, D], F32, "nfrep"); Bnf = Buf()
            ost = [M.alloc([128, D], F32, "ost") for _ in range(2)]; Bost = [Buf(), Buf()]
            junk = M.alloc([128, D], BF16, "junk"); Bjunk = Buf()
            DMA("sp", nfrep[:], nfin_d.partition_broadcast(128), Bnf, None)
            MSET("dve", ssq[:], 0.0, [Bssq])
            for i in range(NT + 2):
                if i < NT:
                    rms_stats(xres[:, i, :], i, [BxT[i][0], BxT[i][1]])
                if i >= 2:
                    i2 = i - 2
                    o = i2 % 2
                    STT("dve", ost[o][:], xres[:, i2, :], rstd[:, i2:i2 + 1], nfrep[:], ALU.mult, ALU.mult, [BxT[i2][0], BxT[i2][1], Brc[i2], Bnf, Bost[o]], [Bost[o]])
                    DMA("sp", out_d[b, i2 * 128:(i2 + 1) * 128, :], ost[o][:], None, Bost[o])
            M.release(xres, garep, *tmp, nfrep, *ost, junk)
            phase_end("fin")

        S.barrier()
        S.emit()
    return nc


def prep_shared(inp):
    f = lambda a: np.ascontiguousarray(np.asarray(a, dtype=np.float32))
    w_in = f(inp["w_in"])[0]
    perm = _perm64()
    groups = []
    for j in range(4):
        cols = []
        qc = np.arange(128 * j, 128 * j + 128)
        pc = np.concatenate([128 * j + perm, 128 * j + 64 + perm])
        cols += [qc, pc, 512 + qc, 512 + pc, 1024 + qc]
        groups.append(w_in[:, np.concatenate(cols)])
    for g in range(4):
        base = 1536
        c = np.arange(128 * g, 128 * g + 128)
        cols = [base + c, base + 512 + c, base + 1024 + c, base + 1536 + c, base + 2048 + c]
        groups.append(w_in[:, np.concatenate(cols)])
    w_ing = np.ascontiguousarray(np.stack(groups, 0))
    ridx, cidx, mask = _na_patterns()
    rpb = f(inp["na_rpb"])[0]
    bg = rpb[:, ridx, cidx]
    bg = bg.transpose(0, 2, 1, 3).reshape(8, 128, NPAT * 128)
    biasg = np.ascontiguousarray(bg.reshape(4, 2, 128, NPAT * 128).transpose(0, 2, 1, 3).reshape(4, 128, 2 * NPAT * 128))
    maskE = np.ascontiguousarray(mask.transpose(1, 0, 2).reshape(128, NPAT * 128))
    C, Sg = _rope_tables()
    lb = f(inp["hg_lb"])
    lbT = np.ascontiguousarray(lb.reshape(2, 2, 4, 128).transpose(3, 2, 1, 0).reshape(128, 16))
    tri = np.zeros((64, 128), np.float32)
    s = np.arange(64)[:, None]
    t = np.arange(64)[None, :]
    tri[:, 0:64] = (s <= t)
    tri[:, 64:128] = (s >= t)
    T8 = lambda v: np.ascontiguousarray(f(v).reshape(-1, 128).T)
    sh = {
        "w_mod": f(inp["w_mod"])[0],
        "b_modT": T8(inp["b_mod"][0]),
        "b_mod_row": f(inp["b_mod"]).reshape(1, 6 * D),
        "nmixT": T8(inp["norm_mix"][0]),
        "nffnT": T8(inp["norm_ffn"][0]),
        "nfin_row": f(inp["norm_final"]).reshape(1, D),
        "w_ing": w_ing,
        "w_out": f(inp["w_out"])[0],
        "biasg": biasg,
        "maskE": maskE,
        "ropeC": C, "ropeS": Sg,
        "lbT": lbT,
        "hgnT": f(inp["hg_norm"])[0].reshape(128, 1),
        "wr": np.ascontiguousarray(np.concatenate([f(inp["w_grp"])[0], f(inp["w_exp"])[0]], axis=1)),
        "br_row": np.concatenate([f(inp["b_grp"])[0], f(inp["b_exp"])[0]]).reshape(1, 20),
        "w1": f(inp["w1"])[0], "w3": f(inp["w3"])[0], "w2": f(inp["w2"])[0],
        "ident": np.eye(128, dtype=np.float32),
        "tri": tri,
    }
    return sh


def core_inputs(inp, sh, bs):
    f = lambda a: np.ascontiguousarray(np.asarray(a, dtype=np.float32))
    c = f(inp["c"])
    cvec = np.stack([c[b] for b in bs] + [f(inp["c_ctx"])] * (3 - len(bs)), 0)
    if len(bs) == 2:
        cvec = np.stack([c[bs[0]], c[bs[1]], f(inp["c_ctx"])], 0)
    else:
        cvec = np.stack([c[bs[0]], c[bs[0]], f(inp["c_ctx"])], 0)
    cT = np.ascontiguousarray(cvec.reshape(3, 8, 128).transpose(2, 1, 0))
    m = dict(sh)
    m["x"] = f(inp["x"])[bs]
    m["ctx"] = f(inp["ctx"])[bs]
    m["cT"] = cT
    return m


_NC_CACHE = {}


def kernel(**inputs):
    sh = prep_shared(inputs)
    if "nc" not in _NC_CACHE:
        _NC_CACHE["nc"] = build(NB=2)
    nc = _NC_CACHE["nc"]
    in_maps = [core_inputs(inputs, sh, [2 * i, 2 * i + 1]) for i in range(8)]
    res = run_bass_kernel_spmd(nc, in_maps, core_ids=list(range(8)))
    out = np.concatenate([np.asarray(r["out"], dtype=np.float32) for r in res.results], axis=0)
    return out
```
